# Optimizing a Trainium2 kernel written in Bass

```python
import math
import jax, jax.numpy as jnp
from jax import lax
import numpy as np

D_MODEL = 1024
BATCH = 2
SEQ = 16384
DEPTH = 2

S5_WIDTH = D_MODEL // 2
S5_GROUP_CH = 16
S5_GROUPS = S5_WIDTH // S5_GROUP_CH
S5_STATE = 64
S5_DT_MIN = 0.001
S5_DT_MAX = 0.1
GLA_WIDTH = D_MODEL - S5_WIDTH
GLA_HEADS = 4
GLA_DV = GLA_WIDTH // GLA_HEADS
GLA_DK = GLA_DV // 2
GLA_KEY = GLA_HEADS * GLA_DK
GLA_GATE_RANK = 16
GLA_TAU = 16.0
GLA_CHUNK = 64
AB_SPLITS = [S5_WIDTH, S5_WIDTH + GLA_KEY, S5_WIDTH + 2 * GLA_KEY,
             S5_WIDTH + 2 * GLA_KEY + GLA_WIDTH, S5_WIDTH + 2 * GLA_KEY + 2 * GLA_WIDTH]
AB_IN = S5_WIDTH + 2 * GLA_KEY + 2 * GLA_WIDTH + 2 * GLA_GATE_RANK
HEAD_DIM = 64
N_HEADS = D_MODEL // HEAD_DIM
N_KV_HEADS = 4
GQA_GROUP = N_HEADS // N_KV_HEADS
ATT_DIM = N_HEADS * HEAD_DIM
KV_DIM = N_KV_HEADS * HEAD_DIM
WINDOW = 128
ATT_BLOCK = 128
ATT_SCALE = HEAD_DIM ** -0.5
REL_BUCKETS = 32
REL_MAX_DIST = 128
NEG_INF = -1e30
N_GROUPS = 4
EXPERTS_PER_GROUP = 8
N_EXPERTS = N_GROUPS * EXPERTS_PER_GROUP
TOP_K = 2
D_EXPERT = 512
MOE_BLOCK = 128
LN_EPS = 1e-5
RMS_EPS = 1e-6
DEEPNORM_ALPHA = (2 * DEPTH) ** 0.25
DEEPNORM_BETA = (8 * DEPTH) ** -0.25
N_EVEN = (DEPTH + 1) // 2
N_ODD = DEPTH // 2

kernel_name = "hybrid_s5_gla_swa_hmoe_deepnorm"


def layer_norm(x, g, b):
    xf = x.astype(jnp.float32)
    mu = jnp.mean(xf, -1, keepdims=True)
    var = jnp.mean(jnp.square(xf - mu), -1, keepdims=True)
    y = (xf - mu) * lax.rsqrt(var + LN_EPS) * g.astype(jnp.float32) + b.astype(jnp.float32)
    return y.astype(x.dtype)


def _complex_linear_combine(e1, e2):
    a1r, a1i, b1r, b1i = e1
    a2r, a2i, b2r, b2i = e2
    ar = a2r * a1r - a2i * a1i
    ai = a2r * a1i + a2i * a1r
    br = a2r * b1r - a2i * b1i + b2r
    bi = a2r * b1i + a2i * b1r + b2i
    return ar, ai, br, bi


def s5_bidir(u, lam_re, lam_im, log_dt, b_re, b_im, c_re, c_im, d, glu_w, glu_b):
    bsz, L, _ = u.shape
    uf = u.astype(jnp.float32).reshape(bsz, L, S5_GROUPS, S5_GROUP_CH)
    y = uf * d.astype(jnp.float32).reshape(S5_GROUPS, S5_GROUP_CH)
    for direction in range(2):
        lr = jnp.minimum(lam_re[direction].astype(jnp.float32), -1e-4)
        li = lam_im[direction].astype(jnp.float32)
        dt = jnp.exp(log_dt[direction].astype(jnp.float32))[:, None]
        mag = jnp.exp(lr * dt)
        ar = mag * jnp.cos(li * dt)
        ai = mag * jnp.sin(li * dt)
        den = lr * lr + li * li
        nr = ar - 1.0
        coef_r = (nr * lr + ai * li) / den
        coef_i = (ai * lr - nr * li) / den
        br_ = b_re[direction].astype(jnp.float32)
        bi_ = b_im[direction].astype(jnp.float32)
        bbr = coef_r[..., None] * br_ - coef_i[..., None] * bi_
        bbi = coef_r[..., None] * bi_ + coef_i[..., None] * br_
        xr = jnp.einsum('blgc,gpc->blgp', uf, bbr)
        xi = jnp.einsum('blgc,gpc->blgp', uf, bbi)
        a_r = jnp.broadcast_to(ar[None, None], (1, L, S5_GROUPS, S5_STATE))
        a_i = jnp.broadcast_to(ai[None, None], (1, L, S5_GROUPS, S5_STATE))
        _, _, hr, hi = lax.associative_scan(_complex_linear_combine, (a_r, a_i, xr, xi),
                                            reverse=(direction == 1), axis=1)
        y = y + (jnp.einsum('blgp,gcp->blgc', hr, c_re[direction].astype(jnp.float32))
                 - jnp.einsum('blgp,gcp->blgc', hi, c_im[direction].astype(jnp.float32)))
    y = jax.nn.gelu(y.reshape(bsz, L, S5_WIDTH))
    y = y * jax.nn.sigmoid(y @ glu_w.astype(jnp.float32) + glu_b.astype(jnp.float32))
    return y.astype(u.dtype)


def gla_chunked(q, k, v, log_a, strict):
    bsz, L, H, dk = q.shape
    dv = v.shape[-1]
    n = L // GLA_CHUNK
    r = lambda t: t.reshape(bsz, n, GLA_CHUNK, H, t.shape[-1])
    q, k, v, log_a = r(q), r(k), r(v), r(log_a)
    bc = jnp.cumsum(log_a, axis=2)
    b_last = bc[:, :, -1]
    qd = q * jnp.exp(bc)
    kd = k * jnp.exp(-bc)
    mask = jnp.tril(jnp.ones((GLA_CHUNK, GLA_CHUNK), bool), k=-1 if strict else 0)
    s = jnp.where(mask, jnp.einsum('bnihd,bnjhd->bnhij', qd, kd), 0.0)
    o_intra = jnp.einsum('bnhij,bnjhe->bnihe', s, v)
    kc = k * jnp.exp(b_last[:, :, None] - bc)
    chunk_kv = jnp.einsum('bnjhd,bnjhe->bnhde', kc, v)
    decay = jnp.exp(b_last)

    def step(state, inp):
        dec, kv = inp
        return state * dec[..., None] + kv, state

    s0 = jnp.zeros((bsz, H, dk, dv), q.dtype)
    _, s_prev = lax.scan(step, s0, (jnp.moveaxis(decay, 1, 0), jnp.moveaxis(chunk_kv, 1, 0)))
    s_prev = jnp.moveaxis(s_prev, 0, 1)
    o_inter = jnp.einsum('bnihd,bnhde->bnihe', qd, s_prev)
    return (o_intra + o_inter).reshape(bsz, L, H, dv)


def gla_bidir(q, k, v, go, lr, gate_w, gate_b, norm_g):
    bsz, L, _ = q.shape
    f32 = jnp.float32
    qf = q.astype(f32).reshape(bsz, L, GLA_HEADS, GLA_DK) * (GLA_DK ** -0.5)
    kf = k.astype(f32).reshape(bsz, L, GLA_HEADS, GLA_DK)
    vf = v.astype(f32).reshape(bsz, L, GLA_HEADS, GLA_DV)
    lrf = lr.astype(f32).reshape(bsz, L, 2, GLA_GATE_RANK)
    z = jnp.einsum('blsr,srk->blsk', lrf, gate_w.astype(f32)) + gate_b.astype(f32)
    log_a = (jax.nn.log_sigmoid(z) / GLA_TAU).reshape(bsz, L, 2, GLA_HEADS, GLA_DK)
    o = gla_chunked(qf, kf, vf, log_a[:, :, 0], strict=False)
    flip = lambda t: jnp.flip(t, axis=1)
    o = o + flip(gla_chunked(flip(qf), flip(kf), flip(vf), flip(log_a[:, :, 1]), strict=True))
    o = o * lax.rsqrt(jnp.mean(o * o, -1, keepdims=True) + RMS_EPS) * norm_g.astype(f32)
    o = o.reshape(bsz, L, GLA_WIDTH) * jax.nn.silu(go.astype(f32))
    return o.astype(q.dtype)


def ssm_gla_mixer(x, w_in, lam_re, lam_im, log_dt, b_re, b_im, c_re, c_im, d, glu_w, glu_b,
                  gate_w, gate_b, norm_g, w_out):
    h = x @ w_in
    u, q, k, v, go, lr = jnp.split(h, AB_SPLITS, axis=-1)
    ya = s5_bidir(u, lam_re, lam_im, log_dt, b_re, b_im, c_re, c_im, d, glu_w, glu_b)
    yb = gla_bidir(q, k, v, go, lr, gate_w, gate_b, norm_g)
    return jnp.concatenate([ya, yb], axis=-1) @ w_out


def t5_bucket(rel):
    nb = REL_BUCKETS // 2
    max_exact = nb // 2
    ret = (rel > 0).astype(jnp.int32) * nb
    n = jnp.abs(rel)
    large = max_exact + (jnp.log(jnp.maximum(n, 1).astype(jnp.float32) / max_exact)
                         / math.log(REL_MAX_DIST / max_exact) * (nb - max_exact)).astype(jnp.int32)
    large = jnp.minimum(large, nb - 1)
    return ret + jnp.where(n < max_exact, n, large)


def window_gqa_mixer(x, w_in, sink, w_out, rel_bias):
    bsz, L, _ = x.shape
    h = x @ w_in
    q, k, v = jnp.split(h, [ATT_DIM, ATT_DIM + KV_DIM], axis=-1)
    k = k.reshape(bsz, L, N_KV_HEADS, HEAD_DIM)
    v = v.reshape(bsz, L, N_KV_HEADS, HEAD_DIM)
    nb = L // ATT_BLOCK
    kw = ATT_BLOCK + 2 * WINDOW
    pad = ((0, 0), (WINDOW, WINDOW), (0, 0), (0, 0))
    kp, vp = jnp.pad(k, pad), jnp.pad(v, pad)
    rel = jnp.arange(kw)[None, :] - WINDOW - jnp.arange(ATT_BLOCK)[:, None]
    band = jnp.abs(rel) <= WINDOW
    bias = rel_bias.astype(jnp.float32)[t5_bucket(rel)]
    bias = jnp.transpose(bias, (2, 0, 1)).reshape(N_KV_HEADS, GQA_GROUP, ATT_BLOCK, kw)
    sink_f = sink.astype(jnp.float32).reshape(N_KV_HEADS, GQA_GROUP)[..., None]
    qb = jnp.moveaxis(q.reshape(bsz, nb, ATT_BLOCK, N_KV_HEADS, GQA_GROUP, HEAD_DIM), 1, 0)

    def attend(args):
        n, qblk = args
        start = n * ATT_BLOCK
        kb = lax.dynamic_slice_in_dim(kp, start, kw, axis=1)
        vb = lax.dynamic_slice_in_dim(vp, start, kw, axis=1)
        kpos = start - WINDOW + jnp.arange(kw)
        valid = band & ((kpos >= 0) & (kpos < L))[None, :]
        s = jnp.einsum('bikgd,bjkd->bkgij', qblk, kb).astype(jnp.float32) * ATT_SCALE + bias
        s = jnp.where(valid, s, NEG_INF)
        m = jnp.maximum(jnp.max(s, -1), sink_f)
        p = jnp.exp(s - m[..., None])
        den = jnp.sum(p, -1) + jnp.exp(sink_f - m)
        o = jnp.einsum('bkgij,bjkd->bkgid', p, vb.astype(jnp.float32)) / den[..., None]
        return jnp.transpose(o, (0, 3, 1, 2, 4)).astype(x.dtype)

    o = lax.map(attend, (jnp.arange(nb), qb))
    o = jnp.moveaxis(o, 0, 1).reshape(bsz, L, ATT_DIM)
    return o @ w_out


def hier_moe(x, w_group, b_group, w_router, b_router, w1, w3, w2):
    bsz, L, D = x.shape
    xt = x.reshape(-1, D)
    T = xt.shape[0]
    tok = jnp.arange(T)
    gp = jax.nn.softmax((xt @ w_group + b_group).astype(jnp.float32), axis=-1)
    grp = jnp.argmax(gp, axis=-1)
    p_grp = gp[tok, grp][:, None]
    el = (xt @ w_router + b_router).astype(jnp.float32).reshape(T, N_GROUPS, EXPERTS_PER_GROUP)
    el = el[tok, grp]
    top_l, top_i = lax.top_k(el, TOP_K)
    gate = jax.nn.softmax(top_l, axis=-1) * p_grp
    eid = grp[:, None] * EXPERTS_PER_GROUP + top_i

    n_assign = T * TOP_K
    fe = eid.reshape(-1)
    ft = jnp.repeat(tok, TOP_K)
    fw = gate.reshape(-1).astype(x.dtype)
    order = jnp.argsort(fe)
    se = fe[order]
    counts = jnp.bincount(fe, length=N_EXPERTS)
    padded = (counts + MOE_BLOCK - 1) // MOE_BLOCK * MOE_BLOCK
    pad_end = jnp.cumsum(padded)
    pad_start = pad_end - padded
    start = jnp.cumsum(counts) - counts
    dest = pad_start[se] + jnp.arange(n_assign) - start[se]
    cap = n_assign + N_EXPERTS * MOE_BLOCK
    nblk = cap // MOE_BLOCK
    buf_tok = jnp.zeros((cap,), jnp.int32).at[dest].set(ft[order].astype(jnp.int32))
    buf_w = jnp.zeros((cap,), x.dtype).at[dest].set(fw[order])
    blk_e = jnp.minimum(jnp.searchsorted(pad_end, jnp.arange(nblk) * MOE_BLOCK, side='right'),
                        N_EXPERTS - 1)

    def expert_block(args):
        t_idx, wt, e = args
        xb = xt[t_idx]
        hdn = jax.nn.silu(xb @ w1[e]) * (xb @ w3[e])
        return (hdn @ w2[e]) * wt[:, None]

    yb = lax.map(expert_block, (buf_tok.reshape(nblk, MOE_BLOCK), buf_w.reshape(nblk, MOE_BLOCK), blk_e))
    y = jnp.zeros_like(xt).at[buf_tok].add(yb.reshape(cap, D))
    return y.reshape(bsz, L, D)


def setup_inputs(seed: int = 0) -> dict:
    key = jax.random.key(seed)
    keys = iter(jax.random.split(key, 48))

    def nrm(shape, scale):
        return jax.random.normal(next(keys), shape, jnp.float32) * scale

    ne, no, nl = N_EVEN, N_ODD, DEPTH
    s5 = (ne, 2, S5_GROUPS, S5_STATE)
    return {
        "x": nrm((BATCH, SEQ, D_MODEL), 1.0),
        "ln_mix_g": 1.0 + nrm((nl, D_MODEL), 0.02),
        "ln_mix_b": nrm((nl, D_MODEL), 0.02),
        "ln_ffn_g": 1.0 + nrm((nl, D_MODEL), 0.02),
        "ln_ffn_b": nrm((nl, D_MODEL), 0.02),
        "ab_w_in": nrm((ne, D_MODEL, AB_IN), D_MODEL ** -0.5),
        "s5_lam_re": -0.5 * jnp.exp(nrm(s5, 0.02)),
        "s5_lam_im": jnp.pi * jnp.arange(S5_STATE, dtype=jnp.float32) + nrm(s5, 0.02),
        "s5_log_dt": jax.random.uniform(next(keys), (ne, 2, S5_GROUPS), jnp.float32,
                                        math.log(S5_DT_MIN), math.log(S5_DT_MAX)),
        "s5_b_re": nrm((ne, 2, S5_GROUPS, S5_STATE, S5_GROUP_CH), (2 * S5_GROUP_CH) ** -0.5),
        "s5_b_im": nrm((ne, 2, S5_GROUPS, S5_STATE, S5_GROUP_CH), (2 * S5_GROUP_CH) ** -0.5),
        "s5_c_re": nrm((ne, 2, S5_GROUPS, S5_GROUP_CH, S5_STATE), (2 * S5_STATE) ** -0.5),
        "s5_c_im": nrm((ne, 2, S5_GROUPS, S5_GROUP_CH, S5_STATE), (2 * S5_STATE) ** -0.5),
        "s5_d": nrm((ne, S5_WIDTH), 1.0),
        "s5_glu_w": nrm((ne, S5_WIDTH, S5_WIDTH), S5_WIDTH ** -0.5),
        "s5_glu_b": nrm((ne, S5_WIDTH), 0.02),
        "gla_gate_w": nrm((ne, 2, GLA_GATE_RANK, GLA_KEY), GLA_GATE_RANK ** -0.5),
        "gla_gate_b": nrm((ne, 2, GLA_KEY), 0.1),
        "gla_norm_g": 1.0 + nrm((ne, GLA_HEADS, GLA_DV), 0.02),
        "ab_w_out": nrm((ne, D_MODEL, D_MODEL), D_MODEL ** -0.5 * DEEPNORM_BETA),
        "c_w_in": nrm((no, D_MODEL, ATT_DIM + 2 * KV_DIM), D_MODEL ** -0.5),
        "c_sink": nrm((no, N_HEADS), 0.5),
        "c_w_out": nrm((no, ATT_DIM, D_MODEL), ATT_DIM ** -0.5 * DEEPNORM_BETA),
        "rel_bias": nrm((REL_BUCKETS, N_HEADS), 0.1),
        "moe_w_group": nrm((nl, D_MODEL, N_GROUPS), D_MODEL ** -0.5),
        "moe_b_group": nrm((nl, N_GROUPS), 0.01),
        "moe_w_router": nrm((nl, D_MODEL, N_EXPERTS), D_MODEL ** -0.5),
        "moe_b_router": nrm((nl, N_EXPERTS), 0.01),
        "moe_w1": nrm((nl, N_EXPERTS, D_MODEL, D_EXPERT), D_MODEL ** -0.5),
        "moe_w3": nrm((nl, N_EXPERTS, D_MODEL, D_EXPERT), D_MODEL ** -0.5),
        "moe_w2": nrm((nl, N_EXPERTS, D_EXPERT, D_MODEL), D_EXPERT ** -0.5 * DEEPNORM_BETA),
    }


def reference(x, ln_mix_g, ln_mix_b, ln_ffn_g, ln_ffn_b, ab_w_in, s5_lam_re, s5_lam_im,
              s5_log_dt, s5_b_re, s5_b_im, s5_c_re, s5_c_im, s5_d, s5_glu_w, s5_glu_b,
              gla_gate_w, gla_gate_b, gla_norm_g, ab_w_out, c_w_in, c_sink, c_w_out, rel_bias,
              moe_w_group, moe_b_group, moe_w_router, moe_b_router, moe_w1, moe_w3, moe_w2):
    h = x
    for layer in range(DEPTH):
        i = layer // 2
        if layer % 2 == 0:
            y = ssm_gla_mixer(h, ab_w_in[i], s5_lam_re[i], s5_lam_im[i], s5_log_dt[i],
                              s5_b_re[i], s5_b_im[i], s5_c_re[i], s5_c_im[i], s5_d[i],
                              s5_glu_w[i], s5_glu_b[i], gla_gate_w[i], gla_gate_b[i],
                              gla_norm_g[i], ab_w_out[i])
        else:
            y = window_gqa_mixer(h, c_w_in[i], c_sink[i], c_w_out[i], rel_bias)
        h = layer_norm(DEEPNORM_ALPHA * h + y, ln_mix_g[layer], ln_mix_b[layer])
        y = hier_moe(h, moe_w_group[layer], moe_b_group[layer], moe_w_router[layer],
                     moe_b_router[layer], moe_w1[layer], moe_w3[layer], moe_w2[layer])
        h = layer_norm(DEEPNORM_ALPHA * h + y, ln_ffn_g[layer], ln_ffn_b[layer])
    return h
```

```python
import math
from contextlib import ExitStack
import numpy as np, time
import concourse.bass as bass
import concourse.mybir as mybir
from concourse.bass_utils import run_bass_kernel_spmd

F32 = mybir.dt.float32
BF16 = mybir.dt.bfloat16
I32 = mybir.dt.int32
AF = mybir.ActivationFunctionType
ALU = mybir.AluOpType
AX = mybir.AxisListType


class Sched:
    def __init__(self, nc, n_dma_sems=24):
        self.nc = nc
        self.eng = {'pe': nc.tensor, 'dve': nc.vector, 'act': nc.scalar,
                    'pool': nc.gpsimd, 'sp': nc.sync}
        self.sem = {k: nc.alloc_semaphore('s_' + k) for k in ('pe', 'dve', 'act', 'pool')}
        self.cnt = {k: 0 for k in self.sem}
        self.dsem = [nc.alloc_semaphore('d%d' % i) for i in range(n_dma_sems)]
        self.dcnt = [0] * n_dma_sems
        self.dnext = 0
        self.waited = {k: {} for k in self.eng}
        self.lastw = {}
        self.readers = {}
        self.nwaits = 0

    def _wait(self, ek, tok):
        if tok is None:
            return
        sem, val, name = tok
        w = self.waited[ek]
        if w.get(name, 0) >= val:
            return
        self.eng[ek].wait_ge(sem, val)
        w[name] = val
        self.nwaits += 1

    @staticmethod
    def _is_psum(k):
        return (isinstance(k, tuple) and k[0] == 'bank') or (isinstance(k, str) and k.startswith('ptr'))

    def _deps(self, ek, r, w, pe_acc=False):
        w = list(w) + [k for k in r if self._is_psum(k)]
        r = [k for k in r if not self._is_psum(k)]
        for k in r:
            self._wait(ek, self.lastw.get(k))
        for k in w:
            lw = self.lastw.get(k)
            if not (pe_acc and lw is not None and lw[2] == 's_pe'):
                self._wait(ek, lw)
            for t in self.readers.get(k, ()):
                self._wait(ek, t)

    def _commit(self, tok, r, w):
        w = list(w) + [k for k in r if self._is_psum(k)]
        r = [k for k in r if not self._is_psum(k)]
        for k in r:
            self.readers.setdefault(k, []).append(tok)
        for k in w:
            self.lastw[k] = tok
            self.readers[k] = []

    def op(self, ek, fn, r=(), w=(), pe_acc=False):
        self._deps(ek, r, w, pe_acc)
        ins = fn(self.eng[ek])
        self.cnt[ek] += 1
        ins.then_inc(self.sem[ek], 1)
        tok = (self.sem[ek], self.cnt[ek], 's_' + ek)
        self._commit(tok, r, w)
        return tok

    def dma(self, ek, fn, r=(), w=(), pre=None):
        self._deps(ek, r, w)
        j = self.dnext
        self.dnext = (self.dnext + 1) % len(self.dsem)
        name = 'd%d' % j
        if self.dcnt[j] > 0:
            self._wait(ek, (self.dsem[j], self.dcnt[j], name))
        if pre is not None:
            pre(self.eng[ek])
        ins = fn(self.eng[ek])
        self.dcnt[j] += 16
        ins.then_inc(self.dsem[j], 16)
        tok = (self.dsem[j], self.dcnt[j], name)
        self._commit(tok, r, w)
        return tok

    def barrier(self):
        for ek in ('pe', 'dve', 'act', 'pool', 'sp'):
            self.wait_all(ek)

    def wait_all(self, ek):
        for k in self.sem:
            if self.cnt[k]:
                self._wait(ek, (self.sem[k], self.cnt[k], 's_' + k))
        for j, s in enumerate(self.dsem):
            if self.dcnt[j]:
                self._wait(ek, (s, self.dcnt[j], 'd%d' % j))


L = 16384
NBLK = L // 512
NCH = L // 128
TWO_PI = 2.0 * math.pi


def KS(name, lo, hi, blk=512):
    return [(name, i) for i in range(lo // blk, (hi - 1) // blk + 1)]


def build_l1(do_gla=True, do_s5=True, ngroups=8, stage=99, nblk_a=NBLK):
    nc = bass.Bass("TRN2", target_bir_lowering=False)
    S = Sched(nc)
    din = lambda n, s, dt=F32: nc.dram_tensor(n, s, dt, kind="ExternalInput").ap()
    xT = din("xT", [1024, L])
    w_fm = din("w_fm", [1024, 416])
    w_tm = din("w_tm", [1024, 256])
    gw = din("gw", [16, 256])
    gb = din("gb", [128, 1])
    ng = din("ng", [128, 128])
    p_l1 = din("p_l1", [128, 5, 128])
    p_l2 = din("p_l2", [128, 3, 16])
    c_l2 = din("c_l2", [128, 16, 16])
    d_l1 = din("d_l1", [128, 1])
    ypreT = nc.dram_tensor("ypreT", [128, L], F32, kind="ExternalOutput").ap()
    yb = nc.dram_tensor("yb", [L, 128], F32, kind="ExternalOutput").ap()
    v_sc = nc.dram_tensor("v_sc", [L, 128], BF16, kind="ExternalOutput").ap()
    sgo_sc = nc.dram_tensor("sgo_sc", [L, 128], BF16, kind="ExternalOutput").ap()

    from contextlib import ExitStack
    gstack = ExitStack()
    sbp = lambda n, s, dt=F32: nc.alloc_sbuf_tensor(n, s, dt)
    sb = lambda n, s, dt=F32: gstack.enter_context(nc.sbuf_tensor(n, s, dt))
    banks = [nc.alloc_psum_tensor("bank%d" % i, [128, 512], F32) for i in range(7)]
    ptr_t = nc.alloc_psum_tensor("ptr_t", [128, 512], BF16)
    BK = lambda i: ('bank', i)

    identf = sbp("identf", [128, 128])
    ident = sbp("ident", [128, 128], BF16)
    E2 = sbp("E2", [128, 64])
    bigA = sbp("bigA", [128, L], BF16)
    bigB = sbp("bigB", [128, L], BF16)
    uT = sbp("uT", [128, L], BF16)
    S.op('pool', lambda e: e.memset(identf[:], 0.0), w=['identf'])
    S.op('pool', lambda e: e.affine_select(out=identf[:], in_=identf[:], pattern=[[-1, 128]], compare_op=ALU.not_equal,
                                           fill=1.0, base=0, channel_multiplier=1), r=['identf'], w=['identf'])
    S.op('dve', lambda e: e.tensor_copy(out=ident[:], in_=identf[:]), r=['identf'], w=['ident'])
    S.op('dve', lambda e: e.tensor_tensor(out=E2[:], in0=identf[:, 0:64], in1=identf[:, 64:128], op=ALU.add), r=['identf'], w=['E2'])
    mask_f = sb("mask_f", [128, 128]); mask_b = sb("mask_b", [128, 128])
    ones = sb("ones", [128, 128])
    S.op('pool', lambda e: e.memset(ones[:], 1.0), w=['ones'])
    S.op('pool', lambda e: e.affine_select(out=mask_f[:], in_=ones[:], pattern=[[1, 128]], compare_op=ALU.is_ge,
                                           fill=0.0, base=0, channel_multiplier=-1), r=['ones'], w=['mask_f'])
    S.op('pool', lambda e: e.affine_select(out=mask_b[:], in_=ones[:], pattern=[[-1, 128]], compare_op=ALU.is_gt,
                                           fill=0.0, base=0, channel_multiplier=1), r=['ones'], w=['mask_b'])
    rmask = sb("rmask", [128, 512])
    S.op('pool', lambda e: e.memset(rmask[:], 1.0), w=['rmask'])
    for c in range(4):
        S.op('pool', lambda e, c=c: e.memset(rmask[:, c * 128:c * 128 + 1], 0.0), r=[], w=['rmask'])

    c_one = sb("c_one", [128, 1]); c_eps = sb("c_eps", [128, 1])
    S.op('pool', lambda e: e.memset(c_one[:], 1.0), w=['c_one'])
    S.op('pool', lambda e: e.memset(c_eps[:], 1e-6), w=['c_eps'])
    wfm = sb("wfm", [128, 8, 416], BF16)
    wtm = sb("wtm", [128, 8, 256], BF16)
    S.dma('pool', lambda e: e.dma_start(out=wfm[:], in_=w_fm.rearrange("(kt p) c -> p kt c", p=128)), w=['wfm'])
    S.dma('pool', lambda e: e.dma_start(out=wtm[:], in_=w_tm.rearrange("(kt p) c -> p kt c", p=128)), w=['wtm'])
    gwb = sb("gwb", [16, 256], BF16)
    S.dma('pool', lambda e: e.dma_start(out=gwb[:], in_=gw[:, :]), w=['gwb'])
    ngb = sb("ngb", [128, 128])
    S.dma('sp', lambda e: e.dma_start(out=ngb[:], in_=ng[:, :]), w=['ngb'])
    ngbias = sb("ngbias", [128, 1])
    S.dma('sp', lambda e: e.dma_start(out=ngbias[:], in_=gb[:, :]), w=['ngbias'])
    S.op('dve', lambda e: e.tensor_scalar(out=ngbias[:], in0=ngbias[:], scalar1=-1.0, scalar2=None, op0=ALU.mult), r=['ngbias'], w=['ngbias'])

    states = sb("states", [128, NCH, 128], BF16)
    eT = sb("eT", [128, NCH])

    xbs = [sb("xb%d" % i, [128, 8, 512], BF16) for i in range(2)]

    lrT = [sb("lrT%d" % i, [16, 512], BF16) for i in range(2)]
    sp_l = sb("sp_l", [128, 512]); cs = sb("cs", [128, 512]); eq = sb("eq", [128, 512]); ek = sb("ek", [128, 512])
    ez = sb("ez", [128, 512])
    vblk = sb("vblk", [128, 4, 128], BF16); sgoblk = sb("sgoblk", [128, 4, 128], BF16)
    kdtm4 = sb("kdtm4", [128, 4, 128], BF16)
    for blk in range(nblk_a):
        t0 = blk * 512
        xb = xbs[blk % 2]; xk = 'xb%d' % (blk % 2)
        S.dma('pool', lambda e: e.dma_start(out=xb[:], in_=xT[:, t0:t0 + 512].rearrange("(kt p) t -> p kt t", p=128)), w=[xk])
        for kt in range(8):
            S.op('pe', lambda e: e.matmul(banks[0][:], lhsT=wfm[:, kt, 0:128], rhs=xb[:, kt, :], start=(kt == 0), stop=(kt == 7)),
                 r=[xk, 'wfm'], w=[BK(0)], pe_acc=(kt > 0))
        S.op('act', lambda e: e.activation(out=uT[:, t0:t0 + 512], in_=banks[0][:], func=AF.Copy), r=[BK(0)], w=KS('uT', t0, t0 + 512))
        if not do_gla:
            continue
        if stage < 2:
            continue
        for di in range(2):
            for kt in range(8):
                S.op('pe', lambda e: e.matmul(banks[1 + di][0:16, :], lhsT=wfm[:, kt, 384 + 16 * di:400 + 16 * di], rhs=xb[:, kt, :],
                                              start=(kt == 0), stop=(kt == 7)), r=[xk, 'wfm'], w=[BK(1 + di)], pe_acc=(kt > 0))
            S.op('act', lambda e: e.activation(out=lrT[di][:], in_=banks[1 + di][0:16, :], func=AF.Copy), r=[BK(1 + di)], w=['lrT%d' % di])
        S.op('pe', lambda e: e.matmul(banks[3][:], lhsT=gwb[:, 0:128], rhs=lrT[0][:], start=True, stop=False), r=['gwb', 'lrT0'], w=[BK(3)])
        S.op('pe', lambda e: e.matmul(banks[3][:], lhsT=gwb[:, 128:256], rhs=lrT[1][:], start=False, stop=True), r=['gwb', 'lrT1'], w=[BK(3)], pe_acc=True)
        S.op('act', lambda e: e.activation(out=ez[:], in_=banks[3][:], func=AF.Exp, bias=ngbias[:, 0:1], scale=-1.0), r=[BK(3), 'ngbias'], w=['ez'])
        S.op('act', lambda e: e.activation(out=sp_l[:], in_=ez[:], func=AF.Ln, bias=c_one[:, 0:1], scale=1.0), r=['ez', 'c_one'], w=['sp_l'])
        S.op('dve', lambda e: e.tensor_tensor_scan(out=cs[0:64, :], data0=rmask[0:64, :], data1=sp_l[0:64, :], initial=0.0,
                                                   op0=ALU.mult, op1=ALU.add), r=['rmask', 'sp_l'], w=['cs'])
        S.op('dve', lambda e: e.tensor_tensor_scan(out=cs[64:128, ::-1], data0=rmask[64:128, :], data1=sp_l[64:128, ::-1], initial=0.0,
                                                   op0=ALU.mult, op1=ALU.add), r=['rmask', 'sp_l'], w=['cs'])
        S.op('act', lambda e: e.activation(out=eq[:], in_=cs[:], func=AF.Exp, scale=-1.0 / 16.0), r=['cs'], w=['eq'])
        S.op('act', lambda e: e.activation(out=ek[:], in_=cs[:], func=AF.Exp, scale=1.0 / 16.0), r=['cs'], w=['ek'])
        S.op('act', lambda e: e.activation(out=eT[0:64, blk * 4:blk * 4 + 4], in_=cs[0:64, 127::128], func=AF.Exp, scale=-1.0 / 16.0), r=['cs'], w=['eT'])
        S.op('act', lambda e: e.activation(out=eT[64:128, blk * 4:blk * 4 + 4], in_=cs[64:128, 0::128], func=AF.Exp, scale=-1.0 / 16.0), r=['cs'], w=['eT'])
        for kt in range(8):
            S.op('pe', lambda e: e.matmul(banks[4][:], lhsT=wfm[:, kt, 128:256], rhs=xb[:, kt, :], start=(kt == 0), stop=(kt == 7)),
                 r=[xk, 'wfm'], w=[BK(4)], pe_acc=(kt > 0))
        S.op('dve', lambda e: e.scalar_tensor_tensor(out=bigA[:, t0:t0 + 512], in0=banks[4][:], scalar=0.125, in1=eq[:], op0=ALU.mult, op1=ALU.mult),
             r=[BK(4), 'eq'], w=KS('bigA', t0, t0 + 512))
        for kt in range(8):
            S.op('pe', lambda e: e.matmul(banks[5][:], lhsT=wfm[:, kt, 256:384], rhs=xb[:, kt, :], start=(kt == 0), stop=(kt == 7)),
                 r=[xk, 'wfm'], w=[BK(5)], pe_acc=(kt > 0))
        S.op('dve', lambda e: e.tensor_tensor(out=bigB[:, t0:t0 + 512], in0=banks[5][:], in1=ek[:], op=ALU.mult),
             r=[BK(5), 'ek'], w=KS('bigB', t0, t0 + 512))
        if stage < 3:
            continue
        for sub in range(4):
            bk = 6
            for kt in range(8):
                S.op('pe', lambda e: e.matmul(banks[bk][:, 0:256], lhsT=xb[:, kt, sub * 128:(sub + 1) * 128], rhs=wtm[:, kt, :],
                                              start=(kt == 0), stop=(kt == 7)), r=[xk, 'wtm'], w=[BK(bk)], pe_acc=(kt > 0))
            S.op('dve', lambda e: e.tensor_copy(out=vblk[:, sub, :], in_=banks[bk][:, 0:128]), r=[BK(bk)], w=[('vblk', sub)])
            S.op('act', lambda e: e.activation(out=sgoblk[:, sub, :], in_=banks[bk][:, 128:256], func=AF.Copy), r=[BK(bk)], w=[('sgoblk', sub)])
        S.dma('sp', lambda e: e.dma_start(out=v_sc[t0:t0 + 512, :].rearrange("(s p) e -> p s e", p=128), in_=vblk[:]),
              r=[('vblk', s_) for s_ in range(4)], w=[('v_sc', blk)])
        S.dma('sp', lambda e: e.dma_start(out=sgo_sc[t0:t0 + 512, :].rearrange("(s p) e -> p s e", p=128), in_=sgoblk[:]),
              r=[('sgoblk', s_) for s_ in range(4)], w=[('sgo_sc', blk)])
        if stage < 4:
            continue
        for sub in range(4):
            S.op('pe', lambda e: e.transpose(ptr_t[:, sub * 128:(sub + 1) * 128], bigB[:, t0 + sub * 128:t0 + (sub + 1) * 128], ident[:]),
                 r=KS('bigB', t0, t0 + 512) + ['ident'], w=['ptr'], pe_acc=(sub > 0))
        S.op('dve', lambda e: e.tensor_copy(out=kdtm4[:], in_=ptr_t[:].rearrange("p (s c) -> p s c", c=128)), r=['ptr'], w=['kdtm4'])
        for sub in range(4):
            n = blk * 4 + sub
            bkc = 3 if sub % 2 == 0 else 1
            S.op('pe', lambda e: e.matmul(banks[bkc][:, 0:128], lhsT=kdtm4[:, sub, :], rhs=vblk[:, sub, :], start=True, stop=True),
                 r=['kdtm4', ('vblk', sub)], w=[BK(bkc)])
            S.op('dve', lambda e: e.tensor_scalar(out=states[:, n, :], in0=banks[bkc][:, 0:128], scalar1=eT[:, n:n + 1], scalar2=None, op0=ALU.mult),
                 r=[BK(bkc), 'eT'], w=[('states', n)])

    if do_gla and stage >= 5:
        runf = sb("runf", [128, 128])
        S.op('dve', lambda e: e.memset(runf[:], 0.0), w=['runf'])
        for i in range(NCH):
            for (lo, hi, n) in ((0, 64, i), (64, 128, NCH - 1 - i)):
                S.op('dve', lambda e: e.scalar_tensor_tensor(out=runf[lo:hi, :], in0=runf[lo:hi, :], scalar=eT[lo:hi, n:n + 1], in1=states[lo:hi, n, :],
                                                             op0=ALU.mult, op1=ALU.add), r=['runf', 'eT', ('states', n)], w=['runf'])
                S.op('pool', lambda e: e.tensor_copy(out=states[lo:hi, n, :], in_=runf[lo:hi, :]), r=['runf'], w=[('states', n)])

        NB_ = 3
        vts = [sb("vt%d" % i, [128, 128], BF16) for i in range(NB_)]
        sgs = [sb("sg%d" % i, [128, 128], BF16) for i in range(NB_)]
        Pf = [sb("Pf%d" % i, [128, 128], BF16) for i in range(NB_)]
        Pb = [sb("Pb%d" % i, [128, 128], BF16) for i in range(NB_)]
        gsg = [sb("gsg%d" % i, [128, 128]) for i in range(NB_)]
        ss = [sb("ss%d" % i, [128, 1]) for i in range(NB_)]
        sq = [sb("sq%d" % i, [128, 128]) for i in range(NB_)]
        junk = [sb("junk%d" % i, [128, 128]) for i in range(NB_)]
        scs = [sb("sc%d" % i, [128, 128], BF16) for i in range(NB_)]
        yo = [sb("yo%d" % i, [128, 128]) for i in range(NB_)]
        import itertools

        def chunk_chain(n):
            c0 = n * 128; b2 = n % NB_
            bk = b2
            S.dma('sp', lambda e: e.dma_start(out=vts[b2][:], in_=v_sc[c0:c0 + 128, :]), r=[('v_sc', n // 4)], w=['vt%d' % b2])
            S.dma('sp', lambda e: e.dma_start(out=sgs[b2][:], in_=sgo_sc[c0:c0 + 128, :]), r=[('sgo_sc', n // 4)], w=['sg%d' % b2])
            kA = KS('bigA', c0, c0 + 128); kB = KS('bigB', c0, c0 + 128)
            sct = scs[b2]; sck = 'sc%d' % b2
            if n > 0:
                S.op('pool', lambda e: e.tensor_copy(out=sct[0:64, :], in_=states[0:64, n - 1, :]), r=[('states', n - 1)], w=[sck])
            else:
                S.op('pool', lambda e: e.memset(sct[0:64, :], 0.0), w=[sck])
            if n < NCH - 1:
                S.op('pool', lambda e: e.tensor_copy(out=sct[64:128, :], in_=states[64:128, n + 1, :]), r=[('states', n + 1)], w=[sck])
            else:
                S.op('pool', lambda e: e.memset(sct[64:128, :], 0.0), w=[sck])
            S.op('pool', lambda e: e.memset(ss[b2][:], 0.0), w=['ss%d' % b2])
            yield
            S.op('pe', lambda e: e.matmul(banks[bk][:, 0:128], lhsT=bigB[0:64, c0:c0 + 128], rhs=bigA[0:64, c0:c0 + 128], start=True, stop=True),
                 r=kA + kB, w=[BK(bk)])
            S.op('pe', lambda e: e.matmul(banks[3 + bk][:, 0:128], lhsT=bigB[64:128, c0:c0 + 128], rhs=bigA[64:128, c0:c0 + 128], start=True, stop=True),
                 r=kA + kB, w=[BK(3 + bk)])
            S.op('act', lambda e: e.activation(out=sq[b2][:], in_=sgs[b2][:], func=AF.Exp, scale=-1.0), r=['sg%d' % b2], w=['sq%d' % b2])
            yield
            S.op('dve', lambda e: e.tensor_tensor(out=Pf[b2][:], in0=banks[bk][:, 0:128], in1=mask_f[:], op=ALU.mult), r=[BK(bk), 'mask_f'], w=['Pf%d' % b2])
            S.op('dve', lambda e: e.tensor_tensor(out=Pb[b2][:], in0=banks[3 + bk][:, 0:128], in1=mask_b[:], op=ALU.mult), r=[BK(3 + bk), 'mask_b'], w=['Pb%d' % b2])
            S.op('pool', lambda e: e.tensor_scalar(out=sq[b2][:], in0=sq[b2][:], scalar1=1.0, scalar2=None, op0=ALU.add), r=['sq%d' % b2], w=['sq%d' % b2])
            yield
            S.op('pe', lambda e: e.matmul(banks[bk][:, 256:384], lhsT=Pf[b2][:], rhs=vts[b2][:], start=True, stop=False), r=['Pf%d' % b2, 'vt%d' % b2], w=[BK(bk)])
            S.op('pe', lambda e: e.matmul(banks[bk][:, 256:384], lhsT=Pb[b2][:], rhs=vts[b2][:], start=False, stop=False), r=['Pb%d' % b2, 'vt%d' % b2], w=[BK(bk)], pe_acc=True)
            S.op('pe', lambda e: e.matmul(banks[bk][:, 256:384], lhsT=bigA[:, c0:c0 + 128], rhs=sct[:], start=False, stop=True),
                 r=kA + [sck], w=[BK(bk)], pe_acc=True)
            S.op('dve', lambda e: e.reciprocal(out=sq[b2][:], in_=sq[b2][:]), r=['sq%d' % b2], w=['sq%d' % b2])
            S.op('pool', lambda e: e.tensor_tensor(out=gsg[b2][:], in0=sgs[b2][:], in1=ngb[:], op=ALU.mult), r=['sg%d' % b2, 'ngb'], w=['gsg%d' % b2])
            yield
            S.op('act', lambda e: e.activation(out=junk[b2][:], in_=banks[bk][:, 256:384], func=AF.Square, accum_out=ss[b2][:, 0:1]), r=[BK(bk), 'ss%d' % b2], w=['junk%d' % b2, 'ss%d' % b2])
            S.op('pool', lambda e: e.tensor_tensor(out=gsg[b2][:], in0=gsg[b2][:], in1=sq[b2][:], op=ALU.mult), r=['gsg%d' % b2, 'sq%d' % b2], w=['gsg%d' % b2])
            yield
            S.op('act', lambda e: e.activation(out=ss[b2][:], in_=ss[b2][:], func=AF.Sqrt, bias=c_eps[:, 0:1], scale=1.0 / 128.0), r=['ss%d' % b2, 'c_eps'], w=['ss%d' % b2])
            yield
            S.op('dve', lambda e: e.reciprocal(out=ss[b2][:], in_=ss[b2][:]), r=['ss%d' % b2], w=['ss%d' % b2])
            S.op('dve', lambda e: e.scalar_tensor_tensor(out=yo[b2][:], in0=banks[bk][:, 256:384], scalar=ss[b2][:, 0:1], in1=gsg[b2][:], op0=ALU.mult, op1=ALU.mult),
                 r=[BK(bk), 'ss%d' % b2, 'gsg%d' % b2], w=['yo%d' % b2])
            yield
            S.dma('sp', lambda e: e.dma_start(out=yb[c0:c0 + 128, :], in_=yo[b2][:]), r=['yo%d' % b2])
            yield

        for n0_ in range(0, NCH if stage >= 6 else 0, NB_):
            for _ in itertools.zip_longest(*[chunk_chain(n0_ + i_) for i_ in range(NB_) if n0_ + i_ < NCH]):
                pass

    if do_s5:
        S.barrier()
        gstack.close()
        build_s5(nc, S, sbp, banks, BK, uT, bigA, bigB, p_l1, p_l2, c_l2, d_l1, ypreT, identf, E2, ngroups)
    S.wait_all('sp')
    print("instr counts", S.cnt, "waits", S.nwaits)
    return nc


def complex_base(S, sb, name, lr_raw, li, logdt, F, keys, alloc=None):
    T = {}
    for nm in ('lrc', 'dt', 'lrdt', 'ang', 'mag', 'ar', 'ai', 't0', 't1', 't2'):
        T[nm] = alloc(F) if alloc else sb(name + '_' + nm, [128, F])
    ti = alloc(F).bitcast(I32) if alloc else sb(name + '_ti', [128, F], I32)
    k = name
    S.op('dve', lambda e: e.tensor_scalar(out=T['lrc'][:], in0=lr_raw, scalar1=-1e-4, scalar2=None, op0=ALU.min), r=keys, w=[k + 'lrc'])
    S.op('act', lambda e: e.activation(out=T['dt'][:], in_=logdt, func=AF.Exp), r=keys, w=[k + 'dt'])
    S.op('dve', lambda e: e.tensor_tensor(out=T['lrdt'][:], in0=T['lrc'][:], in1=T['dt'][:], op=ALU.mult), r=[k + 'lrc', k + 'dt'], w=[k + 'lrdt'])
    S.op('dve', lambda e: e.tensor_tensor(out=T['ang'][:], in0=li, in1=T['dt'][:], op=ALU.mult), r=keys + [k + 'dt'], w=[k + 'ang'])
    S.op('act', lambda e: e.activation(out=T['mag'][:], in_=T['lrdt'][:], func=AF.Exp), r=[k + 'lrdt'], w=[k + 'mag'])
    for (dst, shift) in (('ai', 0.0), ('ar', math.pi / 2)):
        S.op('dve', lambda e: e.tensor_scalar(out=T['t0'][:], in0=T['ang'][:], scalar1=1.0 / TWO_PI, scalar2=shift / TWO_PI + 0.5, op0=ALU.mult, op1=ALU.add),
             r=[k + 'ang'], w=[k + 't0'])
        S.op('dve', lambda e: e.tensor_copy(out=ti[:], in_=T['t0'][:]), r=[k + 't0'], w=[k + 'ti'])
        S.op('dve', lambda e: e.tensor_copy(out=T['t1'][:], in_=ti[:]), r=[k + 'ti'], w=[k + 't1'])
        S.op('dve', lambda e: e.scalar_tensor_tensor(out=T['t2'][:], in0=T['t1'][:], scalar=-TWO_PI, in1=T['ang'][:], op0=ALU.mult, op1=ALU.add),
             r=[k + 't1', k + 'ang'], w=[k + 't2'])
        if shift != 0.0:
            S.op('dve', lambda e: e.tensor_scalar(out=T['t2'][:], in0=T['t2'][:], scalar1=shift, scalar2=None, op0=ALU.add), r=[k + 't2'], w=[k + 't2'])
        S.op('dve', lambda e: e.tensor_scalar(out=T['t0'][:], in0=T['t2'][:], scalar1=-math.pi, scalar2=TWO_PI, op0=ALU.is_lt, op1=ALU.mult), r=[k + 't2'], w=[k + 't0'])
        S.op('dve', lambda e: e.tensor_tensor(out=T['t2'][:], in0=T['t2'][:], in1=T['t0'][:], op=ALU.add), r=[k + 't2', k + 't0'], w=[k + 't2'])
        S.op('dve', lambda e: e.tensor_scalar(out=T['t0'][:], in0=T['t2'][:], scalar1=math.pi, scalar2=-TWO_PI, op0=ALU.is_gt, op1=ALU.mult), r=[k + 't2'], w=[k + 't0'])
        S.op('dve', lambda e: e.tensor_tensor(out=T['t2'][:], in0=T['t2'][:], in1=T['t0'][:], op=ALU.add), r=[k + 't2', k + 't0'], w=[k + 't2'])
        S.op('dve', lambda e: e.tensor_scalar(out=T['t2'][:], in0=T['t2'][:], scalar1=-math.pi, scalar2=math.pi, op0=ALU.max, op1=ALU.min), r=[k + 't2'], w=[k + 't2'])
        S.op('act', lambda e: e.activation(out=T[dst][:], in_=T['t2'][:], func=AF.Sin), r=[k + 't2'], w=[k + dst])
        S.op('dve', lambda e: e.tensor_tensor(out=T[dst][:], in0=T[dst][:], in1=T['mag'][:], op=ALU.mult), r=[k + dst, k + 'mag'], w=[k + dst])
    return T


def build_s5(nc, S, sb, banks, BK, uT, bigA, bigB, p_l1, p_l2, c_l2, d_l1, ypreT, identf, E2, ngroups):
    P1 = sb("P1", [128, 5, 128]); P2 = sb("P2", [128, 3, 16]); C2 = sb("C2", [128, 16, 16]); D1 = sb("D1", [128, 1])
    S.dma('sp', lambda e: e.dma_start(out=P1[:], in_=p_l1[:, :, :]), w=['P1'])
    S.dma('sp', lambda e: e.dma_start(out=P2[:], in_=p_l2[:, :, :]), w=['P2'])
    S.dma('sp', lambda e: e.dma_start(out=C2[:], in_=c_l2[:, :, :]), w=['C2'])
    S.dma('sp', lambda e: e.dma_start(out=D1[:], in_=d_l1[:, :]), w=['D1'])
    T1 = complex_base(S, sb, 'c1', P1[:, 0, :], P1[:, 1, :], P1[:, 2, :], 128, ['P1'])
    den = sb("den", [128, 128]); nr = sb("nr", [128, 128]); cr = sb("cr", [128, 128]); ci = sb("ci", [128, 128]); tA = sb("tA", [128, 128]); tB = sb("tB", [128, 128])
    lrc, ar, ai = T1['lrc'], T1['ar'], T1['ai']
    li1 = P1[:, 1, :]
    tt = lambda o, a, b, op, r, w: S.op('dve', lambda e: e.tensor_tensor(out=o, in0=a, in1=b, op=op), r=r, w=w)
    tt(den[:], lrc[:], lrc[:], ALU.mult, ['c1lrc'], ['den'])
    tt(tA[:], li1, li1, ALU.mult, ['P1'], ['tA'])
    tt(den[:], den[:], tA[:], ALU.add, ['den', 'tA'], ['den'])
    S.op('dve', lambda e: e.reciprocal(out=den[:], in_=den[:]), r=['den'], w=['den'])
    S.op('dve', lambda e: e.tensor_scalar(out=nr[:], in0=ar[:], scalar1=-1.0, scalar2=None, op0=ALU.add), r=['c1ar'], w=['nr'])
    tt(tA[:], nr[:], lrc[:], ALU.mult, ['nr', 'c1lrc'], ['tA'])
    tt(tB[:], ai[:], li1, ALU.mult, ['c1ai', 'P1'], ['tB'])
    tt(cr[:], tA[:], tB[:], ALU.add, ['tA', 'tB'], ['cr'])
    tt(cr[:], cr[:], den[:], ALU.mult, ['cr', 'den'], ['cr'])
    tt(tA[:], ai[:], lrc[:], ALU.mult, ['c1ai', 'c1lrc'], ['tA'])
    tt(tB[:], nr[:], li1, ALU.mult, ['nr', 'P1'], ['tB'])
    tt(ci[:], tA[:], tB[:], ALU.subtract, ['tA', 'tB'], ['ci'])
    tt(ci[:], ci[:], den[:], ALU.mult, ['ci', 'den'], ['ci'])
    Ball = sb("Ball", [128, 2, 2, 64])
    br = P1[:, 3, :]; bi = P1[:, 4, :]
    tt(tA[:], cr[:], br, ALU.mult, ['cr', 'P1'], ['tA'])
    tt(tB[:], ci[:], bi, ALU.mult, ['ci', 'P1'], ['tB'])
    for d in range(2):
        tt(Ball[:, d, 0, :], tA[:, d * 64:(d + 1) * 64], tB[:, d * 64:(d + 1) * 64], ALU.subtract, ['tA', 'tB'], ['Ball'])
    tt(tA[:], cr[:], bi, ALU.mult, ['cr', 'P1'], ['tA'])
    tt(tB[:], ci[:], br, ALU.mult, ['ci', 'P1'], ['tB'])
    for d in range(2):
        tt(Ball[:, d, 1, :], tA[:, d * 64:(d + 1) * 64], tB[:, d * 64:(d + 1) * 64], ALU.add, ['tA', 'tB'], ['Ball'])
    gm = sb("gm", [128, 8])
    S.op('dve', lambda e: e.tensor_reduce(out=gm[:], in_=identf[:].rearrange("p (g c) -> p g c", c=16), axis=AX.X, op=ALU.add), r=['identf'], w=['gm'])
    T2 = complex_base(S, sb, 'c2', P2[:, 0, :], P2[:, 1, :], P2[:, 2, :], 16, ['P2'])
    NLEV = 5
    pw_r = [T2['ar']] + [sb("pwr%d" % l, [128, 16]) for l in range(1, NLEV)]
    pw_i = [T2['ai']] + [sb("pwi%d" % l, [128, 16]) for l in range(1, NLEV)]
    kr = lambda l: 'c2ar' if l == 0 else 'pwr%d' % l
    ki = lambda l: 'c2ai' if l == 0 else 'pwi%d' % l
    sqr = sb("sqr", [128, 16]); sqi = sb("sqi", [128, 16]); sqr2 = sb("sqr2", [128, 16]); sqi2 = sb("sqi2", [128, 16])
    for l in range(1, NLEV):
        src_r, src_i, skr, ski = pw_r[l - 1], pw_i[l - 1], kr(l - 1), ki(l - 1)
        for s_ in range(3):
            if s_ == 2:
                dr, di_, dkr, dki = pw_r[l], pw_i[l], kr(l), ki(l)
            elif s_ == 0:
                dr, di_, dkr, dki = sqr, sqi, 'sqr', 'sqi'
            else:
                dr, di_, dkr, dki = sqr2, sqi2, 'sqr2', 'sqi2'
            tt(tA[:, 0:16], src_r[:], src_r[:], ALU.mult, [skr], ['tA'])
            tt(tB[:, 0:16], src_i[:], src_i[:], ALU.mult, [ski], ['tB'])
            S.op('dve', lambda e: e.scalar_tensor_tensor(out=di_[:], in0=src_r[:], scalar=2.0, in1=src_i[:], op0=ALU.mult, op1=ALU.mult), r=[skr, ski], w=[dki])
            tt(dr[:], tA[:, 0:16], tB[:, 0:16], ALU.subtract, ['tA', 'tB'], [dkr])
            src_r, src_i, skr, ski = dr, di_, dkr, dki
    sA = [sb("sA%d" % l, [128, 16]) for l in range(NLEV)]
    sB = [sb("sB%d" % l, [128, 16]) for l in range(NLEV)]
    for l in range(NLEV):
        S.op('dve', lambda e: e.tensor_copy(out=sA[l][0:64, :], in_=pw_r[l][0:64, :]), r=[kr(l)], w=['sA%d' % l])
        S.op('dve', lambda e: e.tensor_scalar(out=sA[l][64:128, :], in0=pw_i[l][64:128, :], scalar1=-1.0, scalar2=None, op0=ALU.mult), r=[ki(l)], w=['sA%d' % l])
        S.op('dve', lambda e: e.tensor_copy(out=sB[l][0:64, :], in_=pw_i[l][0:64, :]), r=[ki(l)], w=['sB%d' % l])
        S.op('dve', lambda e: e.tensor_copy(out=sB[l][64:128, :], in_=pw_r[l][64:128, :]), r=[kr(l)], w=['sB%d' % l])
    Cb = sb("Cb", [128, 16, 16], BF16)
    S.op('dve', lambda e: e.tensor_copy(out=Cb[0:64], in_=C2[0:64]), r=['C2'], w=['Cb'])
    S.op('dve', lambda e: e.tensor_scalar(out=Cb[64:128], in0=C2[64:128], scalar1=-1.0, scalar2=None, op0=ALU.mult), r=['C2'], w=['Cb'])
    Dpad = sb("Dpad", [128, 128], BF16)
    S.op('dve', lambda e: e.tensor_scalar(out=Dpad[:], in0=identf[:], scalar1=D1[:, 0:1], scalar2=None, op0=ALU.mult), r=['identf', 'D1'], w=['Dpad'])

    X = {0: (bigA, 'bigA'), 1: (bigB, 'bigB')}
    Bpad = [sb("Bpad%d" % d, [128, 128], BF16) for d in range(2)]
    A0 = [sb("A0_%d" % d, [128, 128], BF16) for d in range(2)]
    Al = [[sb("A%d_%d" % (l, d), [128, 128]) for d in range(2)] for l in range(1, NLEV)]
    Hb = [sb("Hb%d" % d, [128, 2048], BF16) for d in range(2)]
    S1 = [sb("S1_%d" % d, [128, 2048]) for d in range(2)]
    S2 = [sb("S2_%d" % d, [128, 256]) for d in range(2)]
    S3 = [sb("S3_%d" % d, [128, 32]) for d in range(2)]
    S4 = [sb("S4_%d" % d, [128, 4]) for d in range(2)]
    S5t = [sb("S5_%d" % d, [128, 1]) for d in range(2)]
    Hf = [sb("Hf%d" % d, [128, 256]) for d in range(2)]
    P4 = [sb("P4_%d" % d, [128, 4]) for d in range(2)]
    P3 = [sb("P3_%d" % d, [128, 32]) for d in range(2)]
    P2_ = [sb("P2_%d" % d, [128, 256]) for d in range(2)]
    P1_ = [sb("P1_%d" % d, [128, 2048]) for d in range(2)]
    P1b = [sb("P1b_%d" % d, [128, 2048], BF16) for d in range(2)]
    ystage = [sb("ystage%d" % i, [16, 512]) for i in range(2)]
    Slev = [None, S1, S2, S3, S4, S5t]
    Plev = [None, P1_, P2_, P3, P4]
    Nlev = [L, 2048, 256, 32, 4, 1]
    import itertools
    for g in range(ngroups):
        def chain(d):
            col = d * 8 + g
            Xt, Xn = X[d]
            sfx = '_%d' % d
            S.op('pool', lambda e: e.tensor_scalar(out=Bpad[d][:], in0=Ball[:, d].rearrange("p r q -> p (r q)"), scalar1=gm[:, g:g + 1], scalar2=None, op0=ALU.mult),
                 r=['Ball', 'gm'], w=['Bpad' + sfx])
            for l in range(NLEV):
                At = A0[d] if l == 0 else Al[l - 1][d]
                ak = 'A%d' % l + sfx
                S.op('pool', lambda e: e.tensor_scalar(out=At[:, 0:64], in0=E2[:], scalar1=sA[l][:, col:col + 1], scalar2=None, op0=ALU.mult), r=['E2', 'sA%d' % l], w=[ak])
                S.op('pool', lambda e: e.tensor_scalar(out=At[:, 64:128], in0=E2[:], scalar1=sB[l][:, col:col + 1], scalar2=None, op0=ALU.mult), r=['E2', 'sB%d' % l], w=[ak])
            for blk in range(NBLK):
                t0 = blk * 512; bk = (blk % 2) if d == 0 else 4
                S.op('pe', lambda e: e.matmul(banks[bk][:], lhsT=Bpad[d][:], rhs=uT[:, t0:t0 + 512], start=True, stop=True),
                     r=['Bpad' + sfx] + KS('uT', t0, t0 + 512), w=[BK(bk)])
                S.op('act', lambda e: e.activation(out=Xt[:, t0:t0 + 512], in_=banks[bk][:], func=AF.Copy), r=[BK(bk)], w=KS(Xn, t0, t0 + 512))
                yield
            order = list(range(8)) if d == 0 else list(range(7, -1, -1))
            for k in range(1, 8):
                for q4 in range(4):
                    n0 = q4 * 512; bk = (q4 % 2 + 2) if d == 0 else (q4 % 2 + 5)
                    xkeys = KS(Xn, n0 * 8, (n0 + 512) * 8)
                    jp, j = order[k - 1], order[k]
                    rhs = Xt[:, n0 * 8 + jp:(n0 + 512) * 8:8] if k == 1 else Hb[d][:, n0:n0 + 512]
                    rk = xkeys if k == 1 else [('Hb' + sfx, q4)]
                    S.op('pe', lambda e: e.matmul(banks[bk][:], lhsT=A0[d][:], rhs=rhs, start=True, stop=True), r=['A0' + sfx] + rk, w=[BK(bk)])
                    if k < 7:
                        S.op('dve', lambda e: e.tensor_tensor(out=Hb[d][:, n0:n0 + 512], in0=banks[bk][:], in1=Xt[:, n0 * 8 + j:(n0 + 512) * 8:8], op=ALU.add),
                             r=[BK(bk)] + xkeys, w=[('Hb' + sfx, q4)])
                    else:
                        S.op('dve', lambda e: e.tensor_tensor(out=S1[d][:, n0:n0 + 512], in0=banks[bk][:], in1=Xt[:, n0 * 8 + j:(n0 + 512) * 8:8], op=ALU.add),
                             r=[BK(bk)] + xkeys, w=[('S1' + sfx, q4)])
                    yield
            for l in range(1, 5):
                R = 8 if l < 4 else 4
                Nn = Nlev[l + 1]
                src = Slev[l][d]; dst = Slev[l + 1][d]
                srck = [('S%d' % l + sfx, i) for i in range(4)] if l == 1 else ['S%d' % l + sfx]
                dstk = ['S%d' % (l + 1) + sfx]
                ordr = list(range(R)) if d == 0 else list(range(R - 1, -1, -1))
                bk = 2 if d == 0 else 5
                for k in range(1, R):
                    jp, j = ordr[k - 1], ordr[k]
                    rhs = src[:, jp::R] if k == 1 else Hf[d][:, 0:Nn]
                    rk = srck if k == 1 else ['Hf' + sfx]
                    S.op('pe', lambda e: e.matmul(banks[bk][:, 0:Nn], lhsT=Al[l - 1][d][:], rhs=rhs, start=True, stop=True), r=['A%d' % l + sfx] + rk, w=[BK(bk)])
                    o = Hf[d][:, 0:Nn] if k < R - 1 else dst[:, 0:Nn]
                    ok = ['Hf' + sfx] if k < R - 1 else dstk
                    S.op('dve', lambda e: e.tensor_tensor(out=o, in0=banks[bk][:, 0:Nn], in1=src[:, j::R], op=ALU.add), r=[BK(bk)] + srck, w=ok)
                    yield
            for l in range(4, 0, -1):
                R = 8 if l < 4 else 4
                Nn = Nlev[l + 1]
                Sl = Slev[l][d]; Pl = Plev[l][d]
                slk = [('S%d' % l + sfx, i) for i in range(4)] if l == 1 else ['S%d' % l + sfx]
                plk = 'P%d' % l + sfx
                ordr = list(range(R)) if d == 0 else list(range(R - 1, -1, -1))
                bk = 3 if d == 0 else 6
                if l == 4:
                    S.op('dve', lambda e: e.memset(Pl[:, ordr[0]:ordr[0] + 1], 0.0), w=[plk])
                else:
                    Pup = Plev[l + 1][d]
                    S.op('dve', lambda e: e.tensor_copy(out=Pl[:, ordr[0]::R], in_=Pup[:, 0:Nn]), r=['P%d' % (l + 1) + sfx], w=[plk])
                for k in range(1, R):
                    jp, j = ordr[k - 1], ordr[k]
                    S.op('pe', lambda e: e.matmul(banks[bk][:, 0:Nn], lhsT=Al[l - 1][d][:], rhs=Pl[:, jp::R], start=True, stop=True), r=['A%d' % l + sfx, plk], w=[BK(bk)])
                    S.op('dve', lambda e: e.tensor_tensor(out=Pl[:, j::R], in0=banks[bk][:, 0:Nn], in1=Sl[:, jp::R], op=ALU.add), r=[BK(bk)] + slk, w=[plk])
                    yield
            S.op('act', lambda e: e.activation(out=P1b[d][:], in_=P1_[d][:], func=AF.Copy), r=['P1' + sfx], w=['P1b' + sfx])
            for k in range(8):
                for q4 in range(4):
                    n0 = q4 * 512; bk = (q4 % 2 + 2) if d == 0 else (q4 % 2 + 5)
                    xkeys = KS(Xn, n0 * 8, (n0 + 512) * 8)
                    j = order[k]
                    if k == 0:
                        rhs = P1b[d][:, n0:n0 + 512]; rk = ['P1b' + sfx]
                    else:
                        jp = order[k - 1]
                        rhs = Xt[:, n0 * 8 + jp:(n0 + 512) * 8:8]; rk = xkeys
                    S.op('pe', lambda e: e.matmul(banks[bk][:], lhsT=A0[d][:], rhs=rhs, start=True, stop=True), r=['A0' + sfx] + rk, w=[BK(bk)])
                    xs = Xt[:, n0 * 8 + j:(n0 + 512) * 8:8]
                    S.op('dve', lambda e: e.tensor_tensor(out=xs, in0=banks[bk][:], in1=xs, op=ALU.add), r=[BK(bk)] + xkeys, w=xkeys)
                    yield
        for _ in itertools.zip_longest(chain(0), chain(1)):
            pass
        for blk in range(NBLK):
            t0 = blk * 512; bk = blk % 2; ys = ystage[blk % 2]; yk = 'ystage%d' % (blk % 2)
            S.op('pe', lambda e: e.matmul(banks[bk][0:16, :], lhsT=Cb[:, 0 * 8 + g, :], rhs=bigA[:, t0:t0 + 512], start=True, stop=False),
                 r=['Cb'] + KS('bigA', t0, t0 + 512), w=[BK(bk)])
            S.op('pe', lambda e: e.matmul(banks[bk][0:16, :], lhsT=Cb[:, 1 * 8 + g, :], rhs=bigB[:, t0:t0 + 512], start=False, stop=False),
                 r=['Cb'] + KS('bigB', t0, t0 + 512), w=[BK(bk)], pe_acc=True)
            S.op('pe', lambda e: e.matmul(banks[bk][0:16, :], lhsT=Dpad[:, 16 * g:16 * g + 16], rhs=uT[:, t0:t0 + 512], start=False, stop=True),
                 r=['Dpad'] + KS('uT', t0, t0 + 512), w=[BK(bk)], pe_acc=True)
            S.op('act', lambda e: e.activation(out=ys[:], in_=banks[bk][0:16, :], func=AF.Copy), r=[BK(bk)], w=[yk])
            S.dma('sp', lambda e: e.dma_start(out=ypreT[16 * g:16 * g + 16, t0:t0 + 512], in_=ys[:]), r=[yk])


def prep_l1_inputs(inp, b, q):
    x = inp["x"]; w_in = inp["ab_w_in"][0]
    xT = np.ascontiguousarray(x[b].T)
    ucols = w_in[:, q * 128:(q + 1) * 128]
    qc = w_in[:, 512 + q * 64:512 + (q + 1) * 64]
    kc = w_in[:, 768 + q * 64:768 + (q + 1) * 64]
    vc = w_in[:, 1024 + q * 128:1024 + (q + 1) * 128]
    goc = w_in[:, 1536 + q * 128:1536 + (q + 1) * 128]
    lrc = w_in[:, 2048:2080]
    w_fm = np.ascontiguousarray(np.concatenate([ucols, qc, qc, kc, kc, lrc], axis=1))
    w_tm = np.ascontiguousarray(np.concatenate([vc, goc], axis=1))
    gwf = inp["gla_gate_w"][0]
    gw = np.zeros((16, 256), np.float32)
    gw[:, 0:64] = gwf[0][:, q * 64:(q + 1) * 64]
    gw[:, 128 + 64:256] = gwf[1][:, q * 64:(q + 1) * 64]
    gbf = inp["gla_gate_b"][0]
    gb = np.concatenate([gbf[0][q * 64:(q + 1) * 64], gbf[1][q * 64:(q + 1) * 64]]).reshape(128, 1).astype(np.float32)
    ng = np.ascontiguousarray(np.broadcast_to(inp["gla_norm_g"][0][q][None, :], (128, 128))).astype(np.float32)
    gs = slice(q * 8, (q + 1) * 8)
    lam_re = inp["s5_lam_re"][0][:, gs]; lam_im = inp["s5_lam_im"][0][:, gs]; log_dt = inp["s5_log_dt"][0][:, gs]
    b_re = inp["s5_b_re"][0][:, gs]; b_im = inp["s5_b_im"][0][:, gs]
    c_re = inp["s5_c_re"][0][:, gs]; c_im = inp["s5_c_im"][0][:, gs]
    def l1_from_gp(a):
        t = np.transpose(a, (1, 0, 2))[:, None, :, :]
        t = np.broadcast_to(t, (8, 16, 2, 64))
        return t.reshape(128, 128)
    ldt_gp = np.broadcast_to(log_dt[:, :, None], (2, 8, 64))
    def l1_from_b(a):
        return np.transpose(a, (1, 3, 0, 2)).reshape(128, 128)
    p_l1 = np.stack([l1_from_gp(lam_re), l1_from_gp(lam_im), l1_from_gp(ldt_gp), l1_from_b(b_re), l1_from_b(b_im)], axis=1)
    def l2_from_gp(a):
        t = np.transpose(a, (2, 0, 1)).reshape(64, 16)
        return np.concatenate([t, t], axis=0)
    p_l2 = np.stack([l2_from_gp(lam_re), l2_from_gp(lam_im), l2_from_gp(ldt_gp)], axis=1)
    c2 = np.concatenate([np.transpose(c_re, (3, 0, 1, 2)).reshape(64, 16, 16), np.transpose(c_im, (3, 0, 1, 2)).reshape(64, 16, 16)], axis=0)
    d_l1 = inp["s5_d"][0][q * 128:(q + 1) * 128].reshape(128, 1)
    f = lambda a: np.ascontiguousarray(a, dtype=np.float32)
    return {"xT": f(xT), "w_fm": f(w_fm), "w_tm": f(w_tm), "gw": f(gw), "gb": f(gb), "ng": f(ng),
            "p_l1": f(p_l1), "p_l2": f(p_l2), "c_l2": f(c2), "d_l1": f(d_l1)}


NT = 4096
NTILE = NT // 128
ALPHA = 4.0 ** 0.25
HALO = 128
NTH = NT + 2 * HALO
NEG = -30000.0


def build_tail(mode, n_exp=32, ngrp_tok=2, p1stage=9, ntl=NTILE, do_fin=True):
    nc = bass.Bass("TRN2", target_bir_lowering=False)
    S = Sched(nc)
    din = lambda n, s, dt=F32: nc.dram_tensor(n, s, dt, kind="ExternalInput").ap()
    dout = lambda n, s, dt=F32: nc.dram_tensor(n, s, dt, kind="ExternalOutput").ap()
    if mode == 'ab':
        ypre = din("ypre", [NT, 512]); ybi = din("ybi", [NT, 512]); xin = din("xin", [NT, 1024])
        glu_w = din("glu_w", [512, 512]); glu_b = din("glu_b", [128, 512])
    else:
        hin = din("hin", [NTH, 1024])
        w_qkv = din("w_qkv", [1024, 1536])
        bias_full = din("bias_full", [128, 16, 384])
        kvalid = din("kvalid", [128, 2, 384])
        sink_bc = din("sink_bc", [128, 16])
    w_out = din("w_out", [1024, 1024])
    ln1 = din("ln1", [128, 2, 1024]); ln2 = din("ln2", [128, 2, 1024])
    w_r = din("w_r", [1024, 36]); b_r = din("b_r", [128, 36])
    w1 = din("w1", [32, 1024, 512]); w3 = din("w3", [32, 1024, 512]); w2 = din("w2", [32, 512, 1024])
    ebase_in = din("ebase_in", [128, 32])
    out = dout("out", [NT, 1024])
    CAP = 384
    Xs = nc.dram_tensor("Xs", [32 * CAP, 1024], BF16).ap()
    Ys = nc.dram_tensor("Ys", [32 * CAP, 1024], F32).ap()
    h1_sc = dout("h1_sc", [NT, 1024])

    sbp = lambda n, s, dt=F32: nc.alloc_sbuf_tensor(n, s, dt)
    banks = [nc.alloc_psum_tensor("bank%d" % i, [128, 512], F32) for i in range(6)]
    ptrs = [nc.alloc_psum_tensor("ptr_t%d" % i, [128, 1024], BF16) for i in range(2)]
    BK = lambda i: ('bank', i)

    identf = sbp("identf", [128, 128]); ident = sbp("ident", [128, 128], BF16)
    DEST = [[sbp("D_%d_%d" % (t_, k_), [128, 1], I32) for k_ in range(2)] for t_ in range(NTILE)]
    GATE = sbp("GATE", [128, NTILE, 2])
    ln2t = sbp("ln2t", [128, 2, 1024])
    c_eps = sbp("c_eps", [128, 1])
    dummy = sbp("fence_dummy", [128, 8])
    fence = lambda e: e.memset(dummy[:], 0.0)
    S.op('pool', lambda e: e.memset(identf[:], 0.0), w=['identf'])
    S.op('pool', lambda e: e.affine_select(out=identf[:], in_=identf[:], pattern=[[-1, 128]], compare_op=ALU.not_equal,
                                           fill=1.0, base=0, channel_multiplier=1), r=['identf'], w=['identf'])
    S.op('dve', lambda e: e.tensor_copy(out=ident[:], in_=identf[:]), r=['identf'], w=['ident'])
    S.op('pool', lambda e: e.memset(c_eps[:], 1e-5), w=['c_eps'])
    S.dma('sp', lambda e: e.dma_start(out=ln2t[:], in_=ln2[:, :, :]), w=['ln2t'])

    gs = ExitStack()
    sb = lambda n, s, dt=F32: gs.enter_context(nc.sbuf_tensor(n, s, dt))
    ln1t = sb("ln1t", [128, 2, 1024]); wo = sb("wo", [128, 8, 1024], BF16); wr = sb("wr", [128, 8, 36], BF16); brt = sb("brt", [128, 36])
    S.dma('sp', lambda e: e.dma_start(out=ln1t[:], in_=ln1[:, :, :]), w=['ln1t'])
    S.dma('pool', lambda e: e.dma_start(out=wo[:], in_=w_out.rearrange("(kt p) c -> p kt c", p=128)), w=['wo'])
    S.dma('pool', lambda e: e.dma_start(out=wr[:], in_=w_r.rearrange("(kt p) c -> p kt c", p=128)), w=['wr'])
    S.dma('sp', lambda e: e.dma_start(out=brt[:], in_=b_r[:, :]), w=['brt'])

    TriS = sb("TriS", [128, 128]); OnesM = sb("OnesM", [128, 128]); carry = sb("carry", [128, 32]); ebase = sb("ebase", [128, 32])
    dsf = sb("dsf", [128, 2])
    S.op('pool', lambda e: e.memset(OnesM[:], 1.0), w=['OnesM'])
    S.op('pool', lambda e: e.affine_select(out=TriS[:], in_=OnesM[:], pattern=[[1, 128]], compare_op=ALU.is_gt, fill=0.0, base=0, channel_multiplier=-1), r=['OnesM'], w=['TriS'])
    S.op('pool', lambda e: e.memset(carry[:], 0.0), w=['carry'])
    S.dma('sp', lambda e: e.dma_start(out=ebase[:], in_=ebase_in[:, :]), w=['ebase'])
    tp_cnt = [0]

    def transpose_to(dst_ap, src_ap, rkeys, wkeys):
        i = tp_cnt[0] % 2; tp_cnt[0] += 1
        pt_ = ptrs[i][:, 0:128]
        S.op('pe', lambda e: e.transpose(pt_, src_ap, ident[:]), r=list(rkeys) + ['ident'], w=['ptr%d' % i])
        eng = 'dve' if (tp_cnt[0] % 2) else 'act'
        if eng == 'dve':
            S.op('dve', lambda e: e.tensor_copy(out=dst_ap, in_=pt_), r=['ptr%d' % i], w=wkeys)
        else:
            S.op('act', lambda e: e.activation(out=dst_ap, in_=pt_, func=AF.Copy), r=['ptr%d' % i], w=wkeys)

    def transpose_group(dst3, srcs, rkeys, wkeys):
        n = len(srcs)
        i = tp_cnt[0] % 2; tp_cnt[0] += 1
        for j, src_ap in enumerate(srcs):
            S.op('pe', lambda e: e.transpose(ptrs[i][:, j * 128:(j + 1) * 128], src_ap, ident[:]), r=list(rkeys) + ['ident'], w=['ptr%d' % i], pe_acc=(j > 0))
        pv = ptrs[i][:, 0:n * 128].rearrange("p (n c) -> p n c", c=128)
        if tp_cnt[0] % 2:
            S.op('dve', lambda e: e.tensor_copy(out=dst3, in_=pv), r=['ptr%d' % i], w=wkeys)
        else:
            S.op('act', lambda e: e.activation(out=dst3, in_=pv, func=AF.Copy), r=['ptr%d' % i], w=wkeys)

    def layer_norm(r_t, rk, lnt, lnk, out_t, ok, tmp, tk, st, stk):
        S.op('pool', lambda e: e.memset(st[:], 0.0), w=[stk])
        S.op('act', lambda e: e.activation(out=tmp[:], in_=r_t[:], func=AF.Copy, accum_out=st[:, 0:1]), r=[rk, stk], w=[tk, stk])
        S.op('dve', lambda e: e.tensor_scalar(out=st[:, 1:2], in0=st[:, 0:1], scalar1=-1.0 / 1024.0, scalar2=None, op0=ALU.mult), r=[stk], w=[stk])
        S.op('dve', lambda e: e.tensor_scalar(out=r_t[:], in0=r_t[:], scalar1=st[:, 1:2], scalar2=None, op0=ALU.add), r=[rk, stk], w=[rk])
        S.op('act', lambda e: e.activation(out=tmp[:], in_=r_t[:], func=AF.Square, accum_out=st[:, 2:3]), r=[rk, stk], w=[tk, stk])
        S.op('act', lambda e: e.activation(out=st[:, 3:4], in_=st[:, 2:3], func=AF.Sqrt, bias=c_eps[:, 0:1], scale=1.0 / 1024.0), r=[stk, 'c_eps'], w=[stk])
        S.op('dve', lambda e: e.reciprocal(out=st[:, 3:4], in_=st[:, 3:4]), r=[stk], w=[stk])
        S.op('dve', lambda e: e.scalar_tensor_tensor(out=tmp[:], in0=r_t[:], scalar=st[:, 3:4], in1=lnt[:, 0, :], op0=ALU.mult, op1=ALU.mult), r=[rk, stk, lnk], w=[tk])
        S.op('pool', lambda e: e.tensor_tensor(out=out_t[:], in0=tmp[:], in1=lnt[:, 1, :], op=ALU.add), r=[tk, lnk], w=[ok])

    if mode == 'ab':
        gw = sb("gw", [128, 4, 512], BF16); gbt = sb("gbt", [128, 512])
        S.dma('pool', lambda e: e.dma_start(out=gw[:], in_=glu_w.rearrange("(kt p) c -> p kt c", p=128)), w=['gw'])
        S.dma('sp', lambda e: e.dma_start(out=gbt[:], in_=glu_b[:, :]), w=['gbt'])
        yp = [sb("yp%d" % i, [128, 512]) for i in range(2)]
        ybt = [sb("ybt%d" % i, [128, 512]) for i in range(2)]
        xt = [sb("xt%d" % i, [128, 1024]) for i in range(2)]
        ya0 = sb("ya0", [128, 512]); ya0b = sb("ya0b", [128, 512], BF16); ya0T = sb("ya0T", [128, 4, 128], BF16)
        sg = sb("sg", [128, 512]); cat = sb("cat", [128, 1024], BF16)
    else:
        wq = sb("wq", [128, 8, 1536], BF16)
        S.dma('pool', lambda e: e.dma_start(out=wq[:], in_=w_qkv.rearrange("(kt p) c -> p kt c", p=128)), w=['wq'])
        biasf = sb("biasf", [128, 16, 384]); kval = sb("kval", [128, 2, 384]); sinkt = sb("sinkt", [128, 16])
        S.dma('sp', lambda e: e.dma_start(out=biasf[:], in_=bias_full[:, :, :]), w=['biasf'])
        S.dma('sp', lambda e: e.dma_start(out=kval[:], in_=kvalid[:, :, :]), w=['kval'])
        S.dma('sp', lambda e: e.dma_start(out=sinkt[:], in_=sink_bc[:, :]), w=['sinkt'])
        kT = sb("kT", [128, 2, NTH], BF16)
        vtm = sb("vtm", [128, NTH // 128, 256], BF16)
        hb = [sb("hb%d" % i, [128, 1024], BF16) for i in range(2)]
        hTt = [sb("hTt%d" % i, [128, 8, 128], BF16) for i in range(2)]
        for t in range(NTH // 128):
            hbt = hb[t % 2]; hk = 'hb%d' % (t % 2); hT_ = hTt[t % 2]; htk = 'hTt%d' % (t % 2)
            S.dma('pool', lambda e: e.dma_start(out=hbt[:], in_=hin[t * 128:(t + 1) * 128, :]), w=[hk])
            transpose_group(hT_[:, :, :], [hbt[:, kt * 128:(kt + 1) * 128] for kt in range(8)], [hk], [htk])
            for pair in range(2):
                bk = pair
                for kt in range(8):
                    S.op('pe', lambda e: e.matmul(banks[bk][:, 0:128], lhsT=wq[:, kt, 1024 + pair * 128:1024 + (pair + 1) * 128], rhs=hT_[:, kt, :],
                                                  start=(kt == 0), stop=(kt == 7)), r=['wq', htk], w=[BK(bk)], pe_acc=(kt > 0))
                S.op('act', lambda e: e.activation(out=kT[:, pair, t * 128:(t + 1) * 128], in_=banks[bk][:, 0:128], func=AF.Copy), r=[BK(bk)], w=[('kT', t)])
            bk = 2 + t % 2
            for kt in range(8):
                S.op('pe', lambda e: e.matmul(banks[bk][:, 0:256], lhsT=hT_[:, kt, :], rhs=wq[:, kt, 1280:1536],
                                              start=(kt == 0), stop=(kt == 7)), r=['wq', htk], w=[BK(bk)], pe_acc=(kt > 0))
            S.op('dve', lambda e: e.tensor_copy(out=vtm[:, t, :], in_=banks[bk][:, 0:256]), r=[BK(bk)], w=[('vtm', t)])
        xt = [sb("xt%d" % i, [128, 1024]) for i in range(2)]
        qTh = [sb("qTh%d" % i, [128, 128], BF16) for i in range(4)]
        sc_t = [sb("sc%d" % i, [128, 384]) for i in range(4)]
        pb = [sb("pb%d" % i, [128, 384], BF16) for i in range(4)]
        pT = [sb("pT%d" % i, [128, 3, 128], BF16) for i in range(4)]
        mx = sb("mx", [128, 16, 4])
        cat = sb("cat", [128, 1024], BF16)
    catT = sb("catT", [128, 8, 128], BF16); h1Tt = sb("h1Tt", [128, 8, 128], BF16)
    rr = [sb("rr%d" % i, [128, 1024]) for i in range(2)]
    tmp = sb("tmp", [128, 1024]); h1f = [sb("h1f%d" % i, [128, 1024]) for i in range(2)]; h1b = sb("h1b", [128, 1024], BF16)
    st = [sb("st%d" % i, [128, 4]) for i in range(2)]
    lg = sb("lg", [128, 36]); rt = sb("rt", [128, 12, 32]); rs = sb("rs", [128, 16])

    for t in range(ntl):
        b2 = t % 2
        r0 = t * 128
        xk = 'xt%d' % b2
        if mode == 'ab':
            S.dma('sp', lambda e: e.dma_start(out=yp[b2][:], in_=ypre[r0:r0 + 128, :]), w=['yp%d' % b2])
            S.dma('sp', lambda e: e.dma_start(out=ybt[b2][:], in_=ybi[r0:r0 + 128, :]), w=['ybt%d' % b2])
            S.dma('sp', lambda e: e.dma_start(out=xt[b2][:], in_=xin[r0:r0 + 128, :]), w=[xk])
            S.op('act', lambda e: e.activation(out=ya0[:], in_=yp[b2][:], func=AF.Gelu_apprx_tanh), r=['yp%d' % b2], w=['ya0'])
            S.op('dve', lambda e: e.tensor_copy(out=ya0b[:], in_=ya0[:]), r=['ya0'], w=['ya0b'])
            transpose_group(ya0T[:, :, :], [ya0b[:, kt * 128:(kt + 1) * 128] for kt in range(4)], ['ya0b'], ['ya0T'])
            for kt in range(4):
                S.op('pe', lambda e: e.matmul(banks[0][:], lhsT=ya0T[:, kt, :], rhs=gw[:, kt, :], start=(kt == 0), stop=(kt == 3)), r=['ya0T', 'gw'], w=[BK(0)], pe_acc=(kt > 0))
            S.op('dve', lambda e: e.tensor_tensor(out=sg[:], in0=banks[0][:], in1=gbt[:], op=ALU.add), r=[BK(0), 'gbt'], w=['sg'])
            S.op('act', lambda e: e.activation(out=sg[:], in_=sg[:], func=AF.Sigmoid), r=['sg'], w=['sg'])
            S.op('dve', lambda e: e.tensor_tensor(out=cat[:, 0:512], in0=ya0[:], in1=sg[:], op=ALU.mult), r=['ya0', 'sg'], w=['cat'])
            S.op('pool', lambda e: e.tensor_copy(out=cat[:, 512:1024], in_=ybt[b2][:]), r=['ybt%d' % b2], w=['cat'])
        else:
            S.dma('sp', lambda e: e.dma_start(out=xt[b2][:], in_=hin[HALO + r0:HALO + r0 + 128, :]), w=[xk])
            k0 = r0
            tq = t + 1
            hbt = hb[t % 2]; hk = 'hb%d' % (t % 2); hT_ = hTt[t % 2]; htk = 'hTt%d' % (t % 2)
            S.dma('pool', lambda e: e.dma_start(out=hbt[:], in_=hin[tq * 128:(tq + 1) * 128, :]), w=[hk])
            transpose_group(hT_[:, :, :], [hbt[:, kt * 128:(kt + 1) * 128] for kt in range(8)], [hk], [htk])
            kkeys = [('kT', tq - 1), ('kT', tq), ('kT', tq + 1)]
            S.op('pool', lambda e: e.memset(mx[:], 0.0), w=[('mx', h_) for h_ in range(16)])
            import itertools

            def head_chain(h):
                hl = h % 4; kv = h // 4; half = kv % 2; pair = kv // 2
                qt = qTh[hl]; qk = 'qTh%d' % hl
                bq = hl
                lo, hi = half * 64, (half + 1) * 64
                c_lo = h * 64 - half * 64
                mk = ('mx', h)
                for kt in range(8):
                    S.op('pe', lambda e: e.matmul(banks[bq][0:hi, 0:128], lhsT=wq[:, kt, c_lo:(h + 1) * 64], rhs=hT_[:, kt, :],
                                                  start=(kt == 0), stop=(kt == 7)), r=['wq', htk], w=[BK(bq)], pe_acc=(kt > 0))
                yield
                S.op('act', lambda e: e.activation(out=qt[lo:hi, :], in_=banks[bq][lo:hi, 0:128], func=AF.Copy, scale=0.125), r=[BK(bq)], w=[qk])
                yield
                S.op('pe', lambda e: e.matmul(banks[bq][:, 128:512], lhsT=qt[lo:hi, :], rhs=kT[lo:hi, pair, k0:k0 + 384], start=True, stop=True), r=[qk] + kkeys, w=[BK(bq)])
                yield
                sct = sc_t[hl]; sk = 'sc%d' % hl
                S.op('dve', lambda e: e.tensor_tensor(out=sct[:], in0=banks[bq][:, 128:512], in1=biasf[:, h, :], op=ALU.add), r=[BK(bq), 'biasf'], w=[sk])
                if t == 0 or t == NTILE - 1:
                    S.op('pool', lambda e: e.tensor_tensor(out=sct[:], in0=sct[:], in1=kval[:, 0 if t == 0 else 1, :], op=ALU.add), r=[sk, 'kval'], w=[sk])
                S.op('dve', lambda e: e.reduce_max(out=mx[:, h, 0:1], in_=sct[:], axis=AX.X), r=[sk], w=[mk])
                S.op('dve', lambda e: e.tensor_tensor(out=mx[:, h, 0:1], in0=mx[:, h, 0:1], in1=sinkt[:, h:h + 1], op=ALU.max), r=[mk, 'sinkt'], w=[mk])
                S.op('dve', lambda e: e.tensor_scalar(out=mx[:, h, 1:2], in0=mx[:, h, 0:1], scalar1=-1.0, scalar2=None, op0=ALU.mult), r=[mk], w=[mk])
                yield
                pbt = pb[hl]; pk = 'pb%d' % hl
                S.op('act', lambda e: e.activation(out=pbt[:], in_=sct[:], func=AF.Exp, bias=mx[:, h, 1:2], scale=1.0, accum_out=mx[:, h, 2:3]), r=[sk, mk], w=[pk, mk])
                S.op('act', lambda e: e.activation(out=mx[:, h, 3:4], in_=sinkt[:, h:h + 1], func=AF.Exp, bias=mx[:, h, 1:2], scale=1.0), r=['sinkt', mk], w=[mk])
                yield
                S.op('dve', lambda e: e.tensor_tensor(out=mx[:, h, 2:3], in0=mx[:, h, 2:3], in1=mx[:, h, 3:4], op=ALU.add), r=[mk], w=[mk])
                S.op('dve', lambda e: e.reciprocal(out=mx[:, h, 2:3], in_=mx[:, h, 2:3]), r=[mk], w=[mk])
                pTt = pT[hl]; ptk = 'pT%d' % hl
                transpose_group(pTt[:, :, :], [pbt[:, j * 128:(j + 1) * 128] for j in range(3)], [pk], [ptk])
                yield
                for j in range(3):
                    S.op('pe', lambda e: e.matmul(banks[bq][:, 0:64], lhsT=pTt[:, j, :], rhs=vtm[:, k0 // 128 + j, kv * 64:(kv + 1) * 64],
                                                  start=(j == 0), stop=(j == 2)), r=[ptk, ('vtm', k0 // 128 + j)], w=[BK(bq)], pe_acc=(j > 0))
                yield
                S.op('dve', lambda e: e.tensor_scalar(out=cat[:, h * 64:(h + 1) * 64], in0=banks[bq][:, 0:64], scalar1=mx[:, h, 2:3], scalar2=None, op0=ALU.mult),
                     r=[BK(bq), mk], w=[('cat', h)])
                yield

            for grp in range(4):
                for _ in itertools.zip_longest(*[head_chain(grp * 4 + i_) for i_ in range(4)]):
                    pass
        if p1stage < 2:
            continue
        catkeys = ['cat'] if mode == 'ab' else [('cat', h_) for h_ in range(16)]
        transpose_group(catT[:, :, :], [cat[:, kt * 128:(kt + 1) * 128] for kt in range(8)], catkeys, ['catT'])
        r_t = rr[b2]; rk = 'rr%d' % b2
        for half in range(2):
            bk = 4 + half
            for kt in range(8):
                S.op('pe', lambda e: e.matmul(banks[bk][:], lhsT=catT[:, kt, :], rhs=wo[:, kt, half * 512:(half + 1) * 512], start=(kt == 0), stop=(kt == 7)),
                     r=['catT', 'wo'], w=[BK(bk)], pe_acc=(kt > 0))
            S.op('dve', lambda e: e.scalar_tensor_tensor(out=r_t[:, half * 512:(half + 1) * 512], in0=xt[b2][:, half * 512:(half + 1) * 512], scalar=ALPHA,
                                                         in1=banks[bk][:], op0=ALU.mult, op1=ALU.add), r=[xk, BK(bk)], w=[rk])
        hf = h1f[b2]; hfk = 'h1f%d' % b2
        layer_norm(r_t, rk, ln1t, 'ln1t', hf, hfk, tmp, 'tmp', st[b2], 'st%d' % b2)
        S.dma('sp', lambda e: e.dma_start(out=h1_sc[r0:r0 + 128, :], in_=hf[:]), r=[hfk], w=[('h1_sc', t)])
        S.op('act', lambda e: e.activation(out=h1b[:], in_=hf[:], func=AF.Copy), r=[hfk], w=['h1b'])
        transpose_group(h1Tt[:, :, :], [h1b[:, kt * 128:(kt + 1) * 128] for kt in range(8)], ['h1b'], ['h1Tt'])
        if p1stage < 3:
            continue
        for kt in range(8):
            S.op('pe', lambda e: e.matmul(banks[4][:, 0:36], lhsT=h1Tt[:, kt, :], rhs=wr[:, kt, :], start=(kt == 0), stop=(kt == 7)),
                 r=['h1Tt', 'wr'], w=[BK(4)], pe_acc=(kt > 0))
        S.op('dve', lambda e: e.tensor_tensor(out=lg[:], in0=banks[4][:, 0:36], in1=brt[:], op=ALU.add), r=[BK(4), 'brt'], w=['lg'])
        dv = lambda fn, r, w: S.op('dve', fn, r=r, w=w)
        pv = lambda fn, r, w: S.op('pool', fn, r=r, w=w)
        dv(lambda e: e.reduce_max(out=rs[:, 0:1], in_=lg[:, 0:4], axis=AX.X), ['lg'], ['rs'])
        pv(lambda e: e.tensor_scalar(out=rs[:, 1:2], in0=rs[:, 0:1], scalar1=-1.0, scalar2=None, op0=ALU.mult), ['rs'], ['rs'])
        S.op('pool', lambda e: e.memset(rs[:, 2:3], 0.0), r=['rs'], w=['rs'])
        S.op('act', lambda e: e.activation(out=rt[:, 0, 0:4], in_=lg[:, 0:4], func=AF.Exp, bias=rs[:, 1:2], scale=1.0, accum_out=rs[:, 2:3]), r=['lg', 'rs'], w=['rt', 'rs'])
        dv(lambda e: e.reciprocal(out=rs[:, 3:4], in_=rs[:, 2:3]), ['rs'], ['rs'])
        pv(lambda e: e.tensor_scalar(out=rt[:, 1, 0:4], in0=lg[:, 0:4], scalar1=rs[:, 0:1], scalar2=None, op0=ALU.is_equal), ['lg', 'rs'], ['rt'])
        pv(lambda e: e.tensor_scalar(out=rt[:, 1, 0:4], in0=rt[:, 1, 0:4], scalar1=-1.0, scalar2=1e9, op0=ALU.add, op1=ALU.mult), ['rt'], ['rt'])
        for g_ in range(4):
            pv(lambda e: e.tensor_scalar(out=rt[:, 2, g_ * 8:(g_ + 1) * 8], in0=lg[:, 4 + g_ * 8:4 + (g_ + 1) * 8], scalar1=rt[:, 1, g_:g_ + 1], scalar2=None, op0=ALU.add), ['lg', 'rt'], ['rt'])
        dv(lambda e: e.reduce_max(out=rs[:, 4:5], in_=rt[:, 2, :], axis=AX.X), ['rt'], ['rs'])
        pv(lambda e: e.tensor_scalar(out=rt[:, 3, :], in0=rt[:, 2, :], scalar1=rs[:, 4:5], scalar2=None, op0=ALU.is_equal), ['rt', 'rs'], ['rt'])
        dv(lambda e: e.scalar_tensor_tensor(out=rt[:, 4, :], in0=rt[:, 3, :], scalar=-1e9, in1=rt[:, 2, :], op0=ALU.mult, op1=ALU.add), ['rt'], ['rt'])
        dv(lambda e: e.reduce_max(out=rs[:, 5:6], in_=rt[:, 4, :], axis=AX.X), ['rt'], ['rs'])
        pv(lambda e: e.tensor_scalar(out=rt[:, 5, :], in0=rt[:, 4, :], scalar1=rs[:, 5:6], scalar2=None, op0=ALU.is_equal), ['rt', 'rs'], ['rt'])
        pv(lambda e: e.tensor_tensor(out=rs[:, 6:7], in0=rs[:, 5:6], in1=rs[:, 4:5], op=ALU.subtract), ['rs'], ['rs'])
        S.op('act', lambda e: e.activation(out=rs[:, 6:7], in_=rs[:, 6:7], func=AF.Exp), r=['rs'], w=['rs'])
        pv(lambda e: e.tensor_scalar(out=rs[:, 6:7], in0=rs[:, 6:7], scalar1=1.0, scalar2=None, op0=ALU.add), ['rs'], ['rs'])
        dv(lambda e: e.reciprocal(out=rs[:, 7:8], in_=rs[:, 6:7]), ['rs'], ['rs'])
        pv(lambda e: e.tensor_tensor(out=rs[:, 7:8], in0=rs[:, 7:8], in1=rs[:, 3:4], op=ALU.mult), ['rs'], ['rs'])
        pv(lambda e: e.tensor_tensor(out=rs[:, 8:9], in0=rs[:, 3:4], in1=rs[:, 7:8], op=ALU.subtract), ['rs'], ['rs'])
        S.op('pool', lambda e: e.tensor_copy(out=GATE[:, t, :], in_=rs[:, 7:9]), r=['rs'], w=[('GATE', t)])
        pv(lambda e: e.tensor_tensor(out=rt[:, 7, :], in0=rt[:, 3, :], in1=rt[:, 5, :], op=ALU.add), ['rt'], ['rt'])
        S.op('pe', lambda e: e.matmul(banks[2][:, 0:32], lhsT=TriS[:], rhs=rt[:, 7, :], start=True, stop=True), r=['TriS', 'rt'], w=[BK(2)])
        S.op('pe', lambda e: e.matmul(banks[3][:, 0:32], lhsT=OnesM[:], rhs=rt[:, 7, :], start=True, stop=True), r=['OnesM', 'rt'], w=[BK(3)])
        dv(lambda e: e.tensor_tensor(out=rt[:, 8, :], in0=banks[2][:, 0:32], in1=carry[:], op=ALU.add), [BK(2), 'carry'], ['rt'])
        pv(lambda e: e.tensor_scalar(out=rt[:, 8, :], in0=rt[:, 8, :], scalar1=float(CAP - 1), scalar2=None, op0=ALU.min), ['rt'], ['rt'])
        pv(lambda e: e.tensor_tensor(out=rt[:, 8, :], in0=rt[:, 8, :], in1=ebase[:], op=ALU.add), ['rt', 'ebase'], ['rt'])
        dv(lambda e: e.tensor_tensor(out=carry[:], in0=carry[:], in1=banks[3][:, 0:32], op=ALU.add), [BK(3), 'carry'], ['carry'])
        pv(lambda e: e.tensor_tensor(out=rt[:, 9, :], in0=rt[:, 8, :], in1=rt[:, 3, :], op=ALU.mult), ['rt'], ['rt'])
        dv(lambda e: e.reduce_sum(out=dsf[:, 0:1], in_=rt[:, 9, :], axis=AX.X), ['rt'], ['dsf'])
        pv(lambda e: e.tensor_tensor(out=rt[:, 10, :], in0=rt[:, 8, :], in1=rt[:, 5, :], op=ALU.mult), ['rt'], ['rt'])
        dv(lambda e: e.reduce_sum(out=dsf[:, 1:2], in_=rt[:, 10, :], axis=AX.X), ['rt'], ['dsf'])
        for k_ in range(2):
            dv(lambda e: e.tensor_copy(out=DEST[t][k_][:], in_=dsf[:, k_:k_ + 1]), ['dsf'], [('DEST', t)])
        for k_ in range(2):
            tk_ = S.dma('pool', lambda e: e.indirect_dma_start(out=Xs[:, :], out_offset=bass.IndirectOffsetOnAxis(ap=DEST[t][k_][:, :], axis=0), in_=h1b[:, :], in_offset=None), r=['h1b', ('DEST', t)], w=['Xs'], pre=fence)
            S._wait('pool', tk_)

    S.barrier()
    gs.close()

    gs2 = ExitStack()
    sb2 = lambda n, s, dt=F32: gs2.enter_context(nc.sbuf_tensor(n, s, dt))
    NST = CAP // 128
    w1b = [sb2("w1b%d" % i, [128, 8, 512], BF16) for i in range(4)]
    w3b = [sb2("w3b%d" % i, [128, 8, 512], BF16) for i in range(4)]
    w2b = [sb2("w2b%d" % i, [128, 4, 1024], BF16) for i in range(4)]
    xe = [sb2("xe%d" % i, [128, NST, 1024], BF16) for i in range(2)]
    xeT = [sb2("xeT%d" % i, [128, 8, CAP], BF16) for i in range(2)]
    sil = [sb2("sil%d" % i, [128, CAP]) for i in range(2)]
    hdT = [sb2("hdT%d" % i, [128, 4, CAP], BF16) for i in range(2)]
    ye = [sb2("ye%d" % i, [128, 1024]) for i in range(2)]
    fin = [sb2("fin%d" % i, [128, 1024]) for i in range(2)]
    y1 = [sb2("y1_%d" % i, [128, 1024]) for i in range(2)]
    y2 = [sb2("y2_%d" % i, [128, 1024]) for i in range(2)]
    fo = [sb2("fo%d" % i, [128, 1024]) for i in range(2)]
    tmp2 = sb2("tmp2", [128, 1024]); st2 = [sb2("st2_%d" % i, [128, 4]) for i in range(2)]
    import itertools

    def expert_chain(ex):
        wb = ex % 4; par = ex % 2
        S.dma('pool', lambda e: e.dma_start(out=w1b[wb][:], in_=w1[ex].rearrange("(kt p) c -> p kt c", p=128)), w=['w1b%d' % wb])
        S.dma('pool', lambda e: e.dma_start(out=w3b[wb][:], in_=w3[ex].rearrange("(kt p) c -> p kt c", p=128)), w=['w3b%d' % wb])
        S.dma('pool', lambda e: e.dma_start(out=w2b[wb][:], in_=w2[ex].rearrange("(kt p) c -> p kt c", p=128)), w=['w2b%d' % wb])
        xet = xe[par]; xek = 'xe%d' % par; xT_ = xeT[par]; xTk = 'xeT%d' % par
        S.dma('sp', lambda e: e.dma_start(out=xet[:], in_=Xs[ex * CAP:(ex + 1) * CAP, :].rearrange("(s p) c -> p s c", p=128)), r=['Xs'], w=[xek])
        yield
        for st_ in range(NST):
            transpose_group(xT_[:, :, st_ * 128:(st_ + 1) * 128], [xet[:, st_, kt * 128:(kt + 1) * 128] for kt in range(8)], [xek], [xTk])
            yield
        hd = hdT[par]; hdk = 'hdT%d' % par
        b1 = 0 + par; b3 = 2 + par; by = 4 + par
        for ht in range(4):
            for kt in range(8):
                S.op('pe', lambda e: e.matmul(banks[b1][:, 0:CAP], lhsT=w1b[wb][:, kt, ht * 128:(ht + 1) * 128], rhs=xT_[:, kt, :], start=(kt == 0), stop=(kt == 7)),
                     r=['w1b%d' % wb, xTk], w=[BK(b1)], pe_acc=(kt > 0))
            yield
            for kt in range(8):
                S.op('pe', lambda e: e.matmul(banks[b3][:, 0:CAP], lhsT=w3b[wb][:, kt, ht * 128:(ht + 1) * 128], rhs=xT_[:, kt, :], start=(kt == 0), stop=(kt == 7)),
                     r=['w3b%d' % wb, xTk], w=[BK(b3)], pe_acc=(kt > 0))
            sl = sil[par]; slk = 'sil%d' % par
            S.op('act', lambda e: e.activation(out=sl[:], in_=banks[b1][:, 0:CAP], func=AF.Silu), r=[BK(b1)], w=[slk])
            yield
            S.op('dve', lambda e: e.tensor_tensor(out=hd[:, ht, :], in0=banks[b3][:, 0:CAP], in1=sl[:], op=ALU.mult), r=[BK(b3), slk], w=[(hdk, ht)])
            yield
        for st_ in range(NST):
            yet = ye[par]; yek = 'ye%d' % par
            for half in range(2):
                for ht in range(4):
                    S.op('pe', lambda e: e.matmul(banks[by][:], lhsT=hd[:, ht, st_ * 128:(st_ + 1) * 128], rhs=w2b[wb][:, ht, half * 512:(half + 1) * 512], start=(ht == 0), stop=(ht == 3)),
                         r=[(hdk, ht), 'w2b%d' % wb], w=[BK(by)], pe_acc=(ht > 0))
                yield
                if half == 0:
                    S.op('dve', lambda e: e.tensor_copy(out=yet[:, 0:512], in_=banks[by][:]), r=[BK(by)], w=[yek])
                else:
                    S.op('act', lambda e: e.activation(out=yet[:, 512:1024], in_=banks[by][:], func=AF.Copy), r=[BK(by)], w=[yek])
                yield
            r0 = ex * CAP + st_ * 128
            S.dma('sp', lambda e: e.dma_start(out=Ys[r0:r0 + 128, :], in_=yet[:]), r=[yek], w=['Ys'])
            yield

    for ex0 in range(0, n_exp, 2):
        for _ in itertools.zip_longest(expert_chain(ex0), expert_chain(ex0 + 1)):
            pass
    S.barrier()
    for t in range(NTILE):
        b2 = t % 2; r0 = t * 128
        S.dma('sp', lambda e: e.dma_start(out=fin[b2][:], in_=h1_sc[r0:r0 + 128, :]), r=[('h1_sc', t)], w=['fin%d' % b2])
        tk_ = S.dma('pool', lambda e: e.indirect_dma_start(out=y1[b2][:, :], out_offset=None, in_=Ys[:, :], in_offset=bass.IndirectOffsetOnAxis(ap=DEST[t][0][:, :], axis=0)), r=['Ys', ('DEST', t)], w=['y1_%d' % b2], pre=fence)
        S._wait('pool', tk_)
        tk_ = S.dma('pool', lambda e: e.indirect_dma_start(out=y2[b2][:, :], out_offset=None, in_=Ys[:, :], in_offset=bass.IndirectOffsetOnAxis(ap=DEST[t][1][:, :], axis=0)), r=['Ys', ('DEST', t)], w=['y2_%d' % b2], pre=fence)
        S._wait('pool', tk_)
        S.op('dve', lambda e: e.scalar_tensor_tensor(out=y1[b2][:], in0=y1[b2][:], scalar=GATE[:, t, 0:1], in1=y1[b2][:], op0=ALU.mult, op1=ALU.bypass),
             r=['y1_%d' % b2, ('GATE', t)], w=['y1_%d' % b2]) if False else None
        S.op('dve', lambda e: e.tensor_scalar(out=y1[b2][:], in0=y1[b2][:], scalar1=GATE[:, t, 0:1], scalar2=None, op0=ALU.mult), r=['y1_%d' % b2, ('GATE', t)], w=['y1_%d' % b2])
        S.op('dve', lambda e: e.scalar_tensor_tensor(out=y1[b2][:], in0=y2[b2][:], scalar=GATE[:, t, 1:2], in1=y1[b2][:], op0=ALU.mult, op1=ALU.add),
             r=['y1_%d' % b2, 'y2_%d' % b2, ('GATE', t)], w=['y1_%d' % b2])
        S.op('dve', lambda e: e.scalar_tensor_tensor(out=fin[b2][:], in0=fin[b2][:], scalar=ALPHA, in1=y1[b2][:], op0=ALU.mult, op1=ALU.add),
             r=['fin%d' % b2, 'y1_%d' % b2], w=['fin%d' % b2])
        layer_norm(fin[b2], 'fin%d' % b2, ln2t, 'ln2t', fo[b2], 'fo%d' % b2, tmp2, 'tmp2', st2[b2], 'st2_%d' % b2)
        S.dma('sp', lambda e: e.dma_start(out=out[r0:r0 + 128, :], in_=fo[b2][:]), r=['fo%d' % b2])
    S.wait_all('sp')
    print("instr counts", S.cnt, "waits", S.nwaits)
    return nc


def bc(a, n=128):
    return np.ascontiguousarray(np.broadcast_to(a[None], (n,) + a.shape)).astype(np.float32)


def common_inputs(inp, layer):
    f = lambda a: np.ascontiguousarray(a, dtype=np.float32)
    return {
        "ln1": f(np.stack([bc(inp["ln_mix_g"][layer]), bc(inp["ln_mix_b"][layer])], axis=1)),
        "ln2": f(np.stack([bc(inp["ln_ffn_g"][layer]), bc(inp["ln_ffn_b"][layer])], axis=1)),
        "w_r": f(np.concatenate([inp["moe_w_group"][layer], inp["moe_w_router"][layer]], axis=1)),
        "b_r": f(bc(np.concatenate([inp["moe_b_group"][layer], inp["moe_b_router"][layer]]))),
        "ebase_in": f(bc(np.arange(32, dtype=np.float32) * 384.0)),
        "w1": f(inp["moe_w1"][layer]), "w3": f(inp["moe_w3"][layer]), "w2": f(inp["moe_w2"][layer]),
    }


def t5_bucket_np(rel):
    nb = 16; max_exact = 8
    ret = (rel > 0).astype(np.int32) * nb
    n = np.abs(rel)
    nf = np.maximum(n, 1).astype(np.float32) / np.float32(max_exact)
    large = max_exact + (np.log(nf).astype(np.float32) / np.float32(math.log(128 / max_exact)) * np.float32(nb - max_exact)).astype(np.int32)
    large = np.minimum(large, nb - 1)
    return ret + np.where(n < max_exact, n, large)


_NC_CACHE = {}


def _get_nc(key, fn):
    if key not in _NC_CACHE:
        _NC_CACHE[key] = fn()
    return _NC_CACHE[key]


def kernel(**inputs):
    inp = {k: np.asarray(v) for k, v in inputs.items()}
    B, Lq = 2, 16384
    nc1 = build_l1()
    maps = [prep_l1_inputs(inp, c // 4, c % 4) for c in range(8)]
    res = run_bass_kernel_spmd(nc1, maps, core_ids=list(range(8)))
    ypre = np.zeros((B, Lq, 512), np.float32); yb = np.zeros((B, Lq, 512), np.float32)
    for c in range(8):
        b, q = c // 4, c % 4
        ypre[b, :, q * 128:(q + 1) * 128] = res.results[c]["ypreT"].T
        yb[b, :, q * 128:(q + 1) * 128] = res.results[c]["yb"]
    del res, maps
    nc2 = build_tail('ab')
    com = common_inputs(inp, 0)
    maps = []
    for c in range(8):
        b, s = c // 4, c % 4
        sl = slice(s * NT, (s + 1) * NT)
        m = dict(com)
        m.update({"ypre": np.ascontiguousarray(ypre[b, sl]), "ybi": np.ascontiguousarray(yb[b, sl]),
                  "xin": np.ascontiguousarray(inp["x"][b, sl], dtype=np.float32),
                  "glu_w": np.ascontiguousarray(inp["s5_glu_w"][0], dtype=np.float32), "glu_b": bc(inp["s5_glu_b"][0]),
                  "w_out": np.ascontiguousarray(inp["ab_w_out"][0], dtype=np.float32)})
        maps.append(m)
    res = run_bass_kernel_spmd(nc2, maps, core_ids=list(range(8)))
    h2 = np.zeros((B, Lq, 1024), np.float32)
    for c in range(8):
        b, s = c // 4, c % 4
        h2[b, s * NT:(s + 1) * NT] = res.results[c]["out"]
    del res, maps, ypre, yb
    nc3 = build_tail('attn')
    com = common_inputs(inp, 1)
    ii = np.arange(128)[:, None]; kk = np.arange(384)[None, :]
    rel = kk - 128 - ii
    bucket = t5_bucket_np(rel)
    rb = inp["rel_bias"].astype(np.float32)[bucket]
    band = (np.abs(rel) <= 128)[:, :, None]
    bias_full = np.where(band, rb, np.float32(NEG)).astype(np.float32).transpose(0, 2, 1)
    bias_full = np.ascontiguousarray(bias_full)
    sink_bc = bc(inp["c_sink"][0].astype(np.float32))
    h2p = np.zeros((B, Lq + 2 * HALO, 1024), np.float32)
    h2p[:, HALO:HALO + Lq] = h2
    maps = []
    for c in range(8):
        b, s = c // 4, c % 4
        m = dict(com)
        kvalid = np.zeros((128, 2, 384), np.float32)
        if s == 0:
            kvalid[:, 0, 0:128] = NEG
        if s == 3:
            kvalid[:, 1, 256:384] = NEG
        m.update({"hin": np.ascontiguousarray(h2p[b, s * NT:s * NT + NTH]), "w_qkv": np.ascontiguousarray(inp["c_w_in"][0], dtype=np.float32),
                  "bias_full": bias_full, "kvalid": kvalid, "sink_bc": sink_bc,
                  "w_out": np.ascontiguousarray(inp["c_w_out"][0], dtype=np.float32)})
        maps.append(m)
    res = run_bass_kernel_spmd(nc3, maps, core_ids=list(range(8)))
    out = np.zeros((B, Lq, 1024), np.float32)
    for c in range(8):
        b, s = c // 4, c % 4
        out[b, s * NT:(s + 1) * NT] = res.results[c]["out"]
    return out
```

```python
import math
from contextlib import ExitStack
import numpy as np, time
import concourse.bass as bass
import concourse.mybir as mybir
from concourse.bass_utils import run_bass_kernel_spmd

F32 = mybir.dt.float32
BF16 = mybir.dt.bfloat16
I32 = mybir.dt.int32
AF = mybir.ActivationFunctionType
ALU = mybir.AluOpType
AX = mybir.AxisListType


class Sched:
    def __init__(self, nc, n_dma_sems=24):
        self.nc = nc
        self.eng = {'pe': nc.tensor, 'dve': nc.vector, 'act': nc.scalar,
                    'pool': nc.gpsimd, 'sp': nc.sync}
        self.sem = {k: nc.alloc_semaphore('s_' + k) for k in ('pe', 'dve', 'act', 'pool')}
        self.cnt = {k: 0 for k in self.sem}
        self.dsem = [nc.alloc_semaphore('d%d' % i) for i in range(n_dma_sems)]
        self.dcnt = [0] * n_dma_sems
        self.dnext = 0
        self.waited = {k: {} for k in self.eng}
        self.lastw = {}
        self.readers = {}
        self.nwaits = 0

    def _wait(self, ek, tok):
        if tok is None:
            return
        sem, val, name = tok
        w = self.waited[ek]
        if w.get(name, 0) >= val:
            return
        self.eng[ek].wait_ge(sem, val)
        w[name] = val
        self.nwaits += 1

    @staticmethod
    def _is_psum(k):
        return (isinstance(k, tuple) and k[0] == 'bank') or (isinstance(k, str) and k.startswith('ptr'))

    def _deps(self, ek, r, w, pe_acc=False):
        w = list(w) + [k for k in r if self._is_psum(k)]
        r = [k for k in r if not self._is_psum(k)]
        for k in r:
            self._wait(ek, self.lastw.get(k))
        for k in w:
            lw = self.lastw.get(k)
            if not (pe_acc and lw is not None and lw[2] == 's_pe'):
                self._wait(ek, lw)
            for t in self.readers.get(k, ()):
                self._wait(ek, t)

    def _commit(self, tok, r, w):
        w = list(w) + [k for k in r if self._is_psum(k)]
        r = [k for k in r if not self._is_psum(k)]
        for k in r:
            self.readers.setdefault(k, []).append(tok)
        for k in w:
            self.lastw[k] = tok
            self.readers[k] = []

    def op(self, ek, fn, r=(), w=(), pe_acc=False):
        self._deps(ek, r, w, pe_acc)
        ins = fn(self.eng[ek])
        self.cnt[ek] += 1
        ins.then_inc(self.sem[ek], 1)
        tok = (self.sem[ek], self.cnt[ek], 's_' + ek)
        self._commit(tok, r, w)
        return tok

    def dma(self, ek, fn, r=(), w=(), pre=None):
        self._deps(ek, r, w)
        j = self.dnext
        self.dnext = (self.dnext + 1) % len(self.dsem)
        name = 'd%d' % j
        if self.dcnt[j] > 0:
            self._wait(ek, (self.dsem[j], self.dcnt[j], name))
        if pre is not None:
            pre(self.eng[ek])
        ins = fn(self.eng[ek])
        self.dcnt[j] += 16
        ins.then_inc(self.dsem[j], 16)
        tok = (self.dsem[j], self.dcnt[j], name)
        self._commit(tok, r, w)
        return tok

    def barrier(self):
        for ek in ('pe', 'dve', 'act', 'pool', 'sp'):
            self.wait_all(ek)

    def wait_all(self, ek):
        for k in self.sem:
            if self.cnt[k]:
                self._wait(ek, (self.sem[k], self.cnt[k], 's_' + k))
        for j, s in enumerate(self.dsem):
            if self.dcnt[j]:
                self._wait(ek, (s, self.dcnt[j], 'd%d' % j))


L = 16384
NBLK = L // 512
NCH = L // 128
TWO_PI = 2.0 * math.pi


def KS(name, lo, hi, blk=512):
    return [(name, i) for i in range(lo // blk, (hi - 1) // blk + 1)]


def build_l1(do_gla=True, do_s5=True, ngroups=8, stage=99, nblk_a=NBLK):
    nc = bass.Bass("TRN2", target_bir_lowering=False)
    S = Sched(nc)
    din = lambda n, s, dt=F32: nc.dram_tensor(n, s, dt, kind="ExternalInput").ap()
    xT = din("xT", [1024, L])
    w_fm = din("w_fm", [1024, 416])
    w_tm = din("w_tm", [1024, 256])
    gw = din("gw", [16, 256])
    gb = din("gb", [128, 1])
    ng = din("ng", [128, 128])
    p_l1 = din("p_l1", [128, 5, 128])
    p_l2 = din("p_l2", [128, 3, 16])
    c_l2 = din("c_l2", [128, 16, 16])
    d_l1 = din("d_l1", [128, 1])
    ypreT = nc.dram_tensor("ypreT", [128, L], F32, kind="ExternalOutput").ap()
    yb = nc.dram_tensor("yb", [L, 128], F32, kind="ExternalOutput").ap()
    v_sc = nc.dram_tensor("v_sc", [L, 128], BF16, kind="ExternalOutput").ap()
    sgo_sc = nc.dram_tensor("sgo_sc", [L, 128], BF16, kind="ExternalOutput").ap()

    from contextlib import ExitStack
    gstack = ExitStack()
    sbp = lambda n, s, dt=F32: nc.alloc_sbuf_tensor(n, s, dt)
    sb = lambda n, s, dt=F32: gstack.enter_context(nc.sbuf_tensor(n, s, dt))
    banks = [nc.alloc_psum_tensor("bank%d" % i, [128, 512], F32) for i in range(7)]
    ptr_t = nc.alloc_psum_tensor("ptr_t", [128, 512], BF16)
    BK = lambda i: ('bank', i)

    identf = sbp("identf", [128, 128])
    ident = sbp("ident", [128, 128], BF16)
    E2 = sbp("E2", [128, 64])
    bigA = sbp("bigA", [128, L], BF16)
    bigB = sbp("bigB", [128, L], BF16)
    uT = sbp("uT", [128, L], BF16)
    S.op('pool', lambda e: e.memset(identf[:], 0.0), w=['identf'])
    S.op('pool', lambda e: e.affine_select(out=identf[:], in_=identf[:], pattern=[[-1, 128]], compare_op=ALU.not_equal,
                                           fill=1.0, base=0, channel_multiplier=1), r=['identf'], w=['identf'])
    S.op('dve', lambda e: e.tensor_copy(out=ident[:], in_=identf[:]), r=['identf'], w=['ident'])
    S.op('dve', lambda e: e.tensor_tensor(out=E2[:], in0=identf[:, 0:64], in1=identf[:, 64:128], op=ALU.add), r=['identf'], w=['E2'])
    mask_f = sb("mask_f", [128, 128]); mask_b = sb("mask_b", [128, 128])
    ones = sb("ones", [128, 128])
    S.op('pool', lambda e: e.memset(ones[:], 1.0), w=['ones'])
    S.op('pool', lambda e: e.affine_select(out=mask_f[:], in_=ones[:], pattern=[[1, 128]], compare_op=ALU.is_ge,
                                           fill=0.0, base=0, channel_multiplier=-1), r=['ones'], w=['mask_f'])
    S.op('pool', lambda e: e.affine_select(out=mask_b[:], in_=ones[:], pattern=[[-1, 128]], compare_op=ALU.is_gt,
                                           fill=0.0, base=0, channel_multiplier=1), r=['ones'], w=['mask_b'])
    rmask = sb("rmask", [128, 512])
    S.op('pool', lambda e: e.memset(rmask[:], 1.0), w=['rmask'])
    for c in range(4):
        S.op('pool', lambda e, c=c: e.memset(rmask[:, c * 128:c * 128 + 1], 0.0), r=[], w=['rmask'])

    c_one = sb("c_one", [128, 1]); c_eps = sb("c_eps", [128, 1])
    S.op('pool', lambda e: e.memset(c_one[:], 1.0), w=['c_one'])
    S.op('pool', lambda e: e.memset(c_eps[:], 1e-6), w=['c_eps'])
    wfm = sb("wfm", [128, 8, 416], BF16)
    wtm = sb("wtm", [128, 8, 256], BF16)
    S.dma('pool', lambda e: e.dma_start(out=wfm[:], in_=w_fm.rearrange("(kt p) c -> p kt c", p=128)), w=['wfm'])
    S.dma('pool', lambda e: e.dma_start(out=wtm[:], in_=w_tm.rearrange("(kt p) c -> p kt c", p=128)), w=['wtm'])
    gwb = sb("gwb", [16, 256], BF16)
    S.dma('pool', lambda e: e.dma_start(out=gwb[:], in_=gw[:, :]), w=['gwb'])
    ngb = sb("ngb", [128, 128])
    S.dma('sp', lambda e: e.dma_start(out=ngb[:], in_=ng[:, :]), w=['ngb'])
    ngbias = sb("ngbias", [128, 1])
    S.dma('sp', lambda e: e.dma_start(out=ngbias[:], in_=gb[:, :]), w=['ngbias'])
    S.op('dve', lambda e: e.tensor_scalar(out=ngbias[:], in0=ngbias[:], scalar1=-1.0, scalar2=None, op0=ALU.mult), r=['ngbias'], w=['ngbias'])

    states = sb("states", [128, NCH, 128], BF16)
    eT = sb("eT", [128, NCH])

    xbs = [sb("xb%d" % i, [128, 8, 512], BF16) for i in range(2)]

    lrT = [sb("lrT%d" % i, [16, 512], BF16) for i in range(2)]
    sp_l = sb("sp_l", [128, 512]); cs = sb("cs", [128, 512]); eq = sb("eq", [128, 512]); ek = sb("ek", [128, 512])
    ez = sb("ez", [128, 512])
    vblk = sb("vblk", [128, 4, 128], BF16); sgoblk = sb("sgoblk", [128, 4, 128], BF16)
    kdtm4 = sb("kdtm4", [128, 4, 128], BF16)
    for blk in range(nblk_a):
        t0 = blk * 512
        xb = xbs[blk % 2]; xk = 'xb%d' % (blk % 2)
        S.dma('pool', lambda e: e.dma_start(out=xb[:], in_=xT[:, t0:t0 + 512].rearrange("(kt p) t -> p kt t", p=128)), w=[xk])
        for kt in range(8):
            S.op('pe', lambda e: e.matmul(banks[0][:], lhsT=wfm[:, kt, 0:128], rhs=xb[:, kt, :], start=(kt == 0), stop=(kt == 7)),
                 r=[xk, 'wfm'], w=[BK(0)], pe_acc=(kt > 0))
        S.op('act', lambda e: e.activation(out=uT[:, t0:t0 + 512], in_=banks[0][:], func=AF.Copy), r=[BK(0)], w=KS('uT', t0, t0 + 512))
        if not do_gla:
            continue
        if stage < 2:
            continue
        for di in range(2):
            for kt in range(8):
                S.op('pe', lambda e: e.matmul(banks[1 + di][0:16, :], lhsT=wfm[:, kt, 384 + 16 * di:400 + 16 * di], rhs=xb[:, kt, :],
                                              start=(kt == 0), stop=(kt == 7)), r=[xk, 'wfm'], w=[BK(1 + di)], pe_acc=(kt > 0))
            S.op('act', lambda e: e.activation(out=lrT[di][:], in_=banks[1 + di][0:16, :], func=AF.Copy), r=[BK(1 + di)], w=['lrT%d' % di])
        S.op('pe', lambda e: e.matmul(banks[3][:], lhsT=gwb[:, 0:128], rhs=lrT[0][:], start=True, stop=False), r=['gwb', 'lrT0'], w=[BK(3)])
        S.op('pe', lambda e: e.matmul(banks[3][:], lhsT=gwb[:, 128:256], rhs=lrT[1][:], start=False, stop=True), r=['gwb', 'lrT1'], w=[BK(3)], pe_acc=True)
        S.op('act', lambda e: e.activation(out=ez[:], in_=banks[3][:], func=AF.Exp, bias=ngbias[:, 0:1], scale=-1.0), r=[BK(3), 'ngbias'], w=['ez'])
        S.op('act', lambda e: e.activation(out=sp_l[:], in_=ez[:], func=AF.Ln, bias=c_one[:, 0:1], scale=1.0), r=['ez', 'c_one'], w=['sp_l'])
        S.op('dve', lambda e: e.tensor_tensor_scan(out=cs[0:64, :], data0=rmask[0:64, :], data1=sp_l[0:64, :], initial=0.0,
                                                   op0=ALU.mult, op1=ALU.add), r=['rmask', 'sp_l'], w=['cs'])
        S.op('dve', lambda e: e.tensor_tensor_scan(out=cs[64:128, ::-1], data0=rmask[64:128, :], data1=sp_l[64:128, ::-1], initial=0.0,
                                                   op0=ALU.mult, op1=ALU.add), r=['rmask', 'sp_l'], w=['cs'])
        S.op('act', lambda e: e.activation(out=eq[:], in_=cs[:], func=AF.Exp, scale=-1.0 / 16.0), r=['cs'], w=['eq'])
        S.op('act', lambda e: e.activation(out=ek[:], in_=cs[:], func=AF.Exp, scale=1.0 / 16.0), r=['cs'], w=['ek'])
        S.op('act', lambda e: e.activation(out=eT[0:64, blk * 4:blk * 4 + 4], in_=cs[0:64, 127::128], func=AF.Exp, scale=-1.0 / 16.0), r=['cs'], w=['eT'])
        S.op('act', lambda e: e.activation(out=eT[64:128, blk * 4:blk * 4 + 4], in_=cs[64:128, 0::128], func=AF.Exp, scale=-1.0 / 16.0), r=['cs'], w=['eT'])
        for kt in range(8):
            S.op('pe', lambda e: e.matmul(banks[4][:], lhsT=wfm[:, kt, 128:256], rhs=xb[:, kt, :], start=(kt == 0), stop=(kt == 7)),
                 r=[xk, 'wfm'], w=[BK(4)], pe_acc=(kt > 0))
        S.op('dve', lambda e: e.scalar_tensor_tensor(out=bigA[:, t0:t0 + 512], in0=banks[4][:], scalar=0.125, in1=eq[:], op0=ALU.mult, op1=ALU.mult),
             r=[BK(4), 'eq'], w=KS('bigA', t0, t0 + 512))
        for kt in range(8):
            S.op('pe', lambda e: e.matmul(banks[5][:], lhsT=wfm[:, kt, 256:384], rhs=xb[:, kt, :], start=(kt == 0), stop=(kt == 7)),
                 r=[xk, 'wfm'], w=[BK(5)], pe_acc=(kt > 0))
        S.op('dve', lambda e: e.tensor_tensor(out=bigB[:, t0:t0 + 512], in0=banks[5][:], in1=ek[:], op=ALU.mult),
             r=[BK(5), 'ek'], w=KS('bigB', t0, t0 + 512))
        if stage < 3:
            continue
        for sub in range(4):
            bk = 6
            for kt in range(8):
                S.op('pe', lambda e: e.matmul(banks[bk][:, 0:256], lhsT=xb[:, kt, sub * 128:(sub + 1) * 128], rhs=wtm[:, kt, :],
                                              start=(kt == 0), stop=(kt == 7)), r=[xk, 'wtm'], w=[BK(bk)], pe_acc=(kt > 0))
            S.op('dve', lambda e: e.tensor_copy(out=vblk[:, sub, :], in_=banks[bk][:, 0:128]), r=[BK(bk)], w=[('vblk', sub)])
            S.op('act', lambda e: e.activation(out=sgoblk[:, sub, :], in_=banks[bk][:, 128:256], func=AF.Copy), r=[BK(bk)], w=[('sgoblk', sub)])
        S.dma('sp', lambda e: e.dma_start(out=v_sc[t0:t0 + 512, :].rearrange("(s p) e -> p s e", p=128), in_=vblk[:]),
              r=[('vblk', s_) for s_ in range(4)], w=[('v_sc', blk)])
        S.dma('sp', lambda e: e.dma_start(out=sgo_sc[t0:t0 + 512, :].rearrange("(s p) e -> p s e", p=128), in_=sgoblk[:]),
              r=[('sgoblk', s_) for s_ in range(4)], w=[('sgo_sc', blk)])
        if stage < 4:
            continue
        for sub in range(4):
            S.op('pe', lambda e: e.transpose(ptr_t[:, sub * 128:(sub + 1) * 128], bigB[:, t0 + sub * 128:t0 + (sub + 1) * 128], ident[:]),
                 r=KS('bigB', t0, t0 + 512) + ['ident'], w=['ptr'], pe_acc=(sub > 0))
        S.op('dve', lambda e: e.tensor_copy(out=kdtm4[:], in_=ptr_t[:].rearrange("p (s c) -> p s c", c=128)), r=['ptr'], w=['kdtm4'])
        for sub in range(4):
            n = blk * 4 + sub
            bkc = 3 if sub % 2 == 0 else 1
            S.op('pe', lambda e: e.matmul(banks[bkc][:, 0:128], lhsT=kdtm4[:, sub, :], rhs=vblk[:, sub, :], start=True, stop=True),
                 r=['kdtm4', ('vblk', sub)], w=[BK(bkc)])
            S.op('dve', lambda e: e.tensor_scalar(out=states[:, n, :], in0=banks[bkc][:, 0:128], scalar1=eT[:, n:n + 1], scalar2=None, op0=ALU.mult),
                 r=[BK(bkc), 'eT'], w=[('states', n)])

    if do_gla and stage >= 5:
        runf = sb("runf", [128, 128])
        S.op('dve', lambda e: e.memset(runf[:], 0.0), w=['runf'])
        for i in range(NCH):
            for (lo, hi, n) in ((0, 64, i), (64, 128, NCH - 1 - i)):
                S.op('dve', lambda e: e.scalar_tensor_tensor(out=runf[lo:hi, :], in0=runf[lo:hi, :], scalar=eT[lo:hi, n:n + 1], in1=states[lo:hi, n, :],
                                                             op0=ALU.mult, op1=ALU.add), r=['runf', 'eT', ('states', n)], w=['runf'])
                S.op('pool', lambda e: e.tensor_copy(out=states[lo:hi, n, :], in_=runf[lo:hi, :]), r=['runf'], w=[('states', n)])

        NB_ = 3
        vts = [sb("vt%d" % i, [128, 128], BF16) for i in range(NB_)]
        sgs = [sb("sg%d" % i, [128, 128], BF16) for i in range(NB_)]
        Pf = [sb("Pf%d" % i, [128, 128], BF16) for i in range(NB_)]
        Pb = [sb("Pb%d" % i, [128, 128], BF16) for i in range(NB_)]
        gsg = [sb("gsg%d" % i, [128, 128]) for i in range(NB_)]
        ss = [sb("ss%d" % i, [128, 1]) for i in range(NB_)]
        sq = [sb("sq%d" % i, [128, 128]) for i in range(NB_)]
        junk = [sb("junk%d" % i, [128, 128]) for i in range(NB_)]
        scs = [sb("sc%d" % i, [128, 128], BF16) for i in range(NB_)]
        yo = [sb("yo%d" % i, [128, 128]) for i in range(NB_)]
        import itertools

        def chunk_chain(n):
            c0 = n * 128; b2 = n % NB_
            bk = b2
            S.dma('sp', lambda e: e.dma_start(out=vts[b2][:], in_=v_sc[c0:c0 + 128, :]), r=[('v_sc', n // 4)], w=['vt%d' % b2])
            S.dma('sp', lambda e: e.dma_start(out=sgs[b2][:], in_=sgo_sc[c0:c0 + 128, :]), r=[('sgo_sc', n // 4)], w=['sg%d' % b2])
            kA = KS('bigA', c0, c0 + 128); kB = KS('bigB', c0, c0 + 128)
            sct = scs[b2]; sck = 'sc%d' % b2
            if n > 0:
                S.op('pool', lambda e: e.tensor_copy(out=sct[0:64, :], in_=states[0:64, n - 1, :]), r=[('states', n - 1)], w=[sck])
            else:
                S.op('pool', lambda e: e.memset(sct[0:64, :], 0.0), w=[sck])
            if n < NCH - 1:
                S.op('pool', lambda e: e.tensor_copy(out=sct[64:128, :], in_=states[64:128, n + 1, :]), r=[('states', n + 1)], w=[sck])
            else:
                S.op('pool', lambda e: e.memset(sct[64:128, :], 0.0), w=[sck])
            S.op('pool', lambda e: e.memset(ss[b2][:], 0.0), w=['ss%d' % b2])
            yield
            S.op('pe', lambda e: e.matmul(banks[bk][:, 0:128], lhsT=bigB[0:64, c0:c0 + 128], rhs=bigA[0:64, c0:c0 + 128], start=True, stop=True),
                 r=kA + kB, w=[BK(bk)])
            S.op('pe', lambda e: e.matmul(banks[3 + bk][:, 0:128], lhsT=bigB[64:128, c0:c0 + 128], rhs=bigA[64:128, c0:c0 + 128], start=True, stop=True),
                 r=kA + kB, w=[BK(3 + bk)])
            S.op('act', lambda e: e.activation(out=sq[b2][:], in_=sgs[b2][:], func=AF.Exp, scale=-1.0), r=['sg%d' % b2], w=['sq%d' % b2])
            yield
            S.op('dve', lambda e: e.tensor_tensor(out=Pf[b2][:], in0=banks[bk][:, 0:128], in1=mask_f[:], op=ALU.mult), r=[BK(bk), 'mask_f'], w=['Pf%d' % b2])
            S.op('dve', lambda e: e.tensor_tensor(out=Pb[b2][:], in0=banks[3 + bk][:, 0:128], in1=mask_b[:], op=ALU.mult), r=[BK(3 + bk), 'mask_b'], w=['Pb%d' % b2])
            S.op('pool', lambda e: e.tensor_scalar(out=sq[b2][:], in0=sq[b2][:], scalar1=1.0, scalar2=None, op0=ALU.add), r=['sq%d' % b2], w=['sq%d' % b2])
            yield
            S.op('pe', lambda e: e.matmul(banks[bk][:, 256:384], lhsT=Pf[b2][:], rhs=vts[b2][:], start=True, stop=False), r=['Pf%d' % b2, 'vt%d' % b2], w=[BK(bk)])
            S.op('pe', lambda e: e.matmul(banks[bk][:, 256:384], lhsT=Pb[b2][:], rhs=vts[b2][:], start=False, stop=False), r=['Pb%d' % b2, 'vt%d' % b2], w=[BK(bk)], pe_acc=True)
            S.op('pe', lambda e: e.matmul(banks[bk][:, 256:384], lhsT=bigA[:, c0:c0 + 128], rhs=sct[:], start=False, stop=True),
                 r=kA + [sck], w=[BK(bk)], pe_acc=True)
            S.op('dve', lambda e: e.reciprocal(out=sq[b2][:], in_=sq[b2][:]), r=['sq%d' % b2], w=['sq%d' % b2])
            S.op('pool', lambda e: e.tensor_tensor(out=gsg[b2][:], in0=sgs[b2][:], in1=ngb[:], op=ALU.mult), r=['sg%d' % b2, 'ngb'], w=['gsg%d' % b2])
            yield
            S.op('act', lambda e: e.activation(out=junk[b2][:], in_=banks[bk][:, 256:384], func=AF.Square, accum_out=ss[b2][:, 0:1]), r=[BK(bk), 'ss%d' % b2], w=['junk%d' % b2, 'ss%d' % b2])
            S.op('pool', lambda e: e.tensor_tensor(out=gsg[b2][:], in0=gsg[b2][:], in1=sq[b2][:], op=ALU.mult), r=['gsg%d' % b2, 'sq%d' % b2], w=['gsg%d' % b2])
            yield
            S.op('act', lambda e: e.activation(out=ss[b2][:], in_=ss[b2][:], func=AF.Sqrt, bias=c_eps[:, 0:1], scale=1.0 / 128.0), r=['ss%d' % b2, 'c_eps'], w=['ss%d' % b2])
            yield
            S.op('dve', lambda e: e.reciprocal(out=ss[b2][:], in_=ss[b2][:]), r=['ss%d' % b2], w=['ss%d' % b2])
            S.op('dve', lambda e: e.scalar_tensor_tensor(out=yo[b2][:], in0=banks[bk][:, 256:384], scalar=ss[b2][:, 0:1], in1=gsg[b2][:], op0=ALU.mult, op1=ALU.mult),
                 r=[BK(bk), 'ss%d' % b2, 'gsg%d' % b2], w=['yo%d' % b2])
            yield
            S.dma('sp', lambda e: e.dma_start(out=yb[c0:c0 + 128, :], in_=yo[b2][:]), r=['yo%d' % b2])
            yield

        for n0_ in range(0, NCH if stage >= 6 else 0, NB_):
            for _ in itertools.zip_longest(*[chunk_chain(n0_ + i_) for i_ in range(NB_) if n0_ + i_ < NCH]):
                pass

    if do_s5:
        S.barrier()
        gstack.close()
        build_s5(nc, S, sbp, banks, BK, uT, bigA, bigB, p_l1, p_l2, c_l2, d_l1, ypreT, identf, E2, ngroups)
    S.wait_all('sp')
    print("instr counts", S.cnt, "waits", S.nwaits)
    return nc


def complex_base(S, sb, name, lr_raw, li, logdt, F, keys, alloc=None):
    T = {}
    for nm in ('lrc', 'dt', 'lrdt', 'ang', 'mag', 'ar', 'ai', 't0', 't1', 't2'):
        T[nm] = alloc(F) if alloc else sb(name + '_' + nm, [128, F])
    ti = alloc(F).bitcast(I32) if alloc else sb(name + '_ti', [128, F], I32)
    k = name
    S.op('dve', lambda e: e.tensor_scalar(out=T['lrc'][:], in0=lr_raw, scalar1=-1e-4, scalar2=None, op0=ALU.min), r=keys, w=[k + 'lrc'])
    S.op('act', lambda e: e.activation(out=T['dt'][:], in_=logdt, func=AF.Exp), r=keys, w=[k + 'dt'])
    S.op('dve', lambda e: e.tensor_tensor(out=T['lrdt'][:], in0=T['lrc'][:], in1=T['dt'][:], op=ALU.mult), r=[k + 'lrc', k + 'dt'], w=[k + 'lrdt'])
    S.op('dve', lambda e: e.tensor_tensor(out=T['ang'][:], in0=li, in1=T['dt'][:], op=ALU.mult), r=keys + [k + 'dt'], w=[k + 'ang'])
    S.op('act', lambda e: e.activation(out=T['mag'][:], in_=T['lrdt'][:], func=AF.Exp), r=[k + 'lrdt'], w=[k + 'mag'])
    for (dst, shift) in (('ai', 0.0), ('ar', math.pi / 2)):
        S.op('dve', lambda e: e.tensor_scalar(out=T['t0'][:], in0=T['ang'][:], scalar1=1.0 / TWO_PI, scalar2=shift / TWO_PI + 0.5, op0=ALU.mult, op1=ALU.add),
             r=[k + 'ang'], w=[k + 't0'])
        S.op('dve', lambda e: e.tensor_copy(out=ti[:], in_=T['t0'][:]), r=[k + 't0'], w=[k + 'ti'])
        S.op('dve', lambda e: e.tensor_copy(out=T['t1'][:], in_=ti[:]), r=[k + 'ti'], w=[k + 't1'])
        S.op('dve', lambda e: e.scalar_tensor_tensor(out=T['t2'][:], in0=T['t1'][:], scalar=-TWO_PI, in1=T['ang'][:], op0=ALU.mult, op1=ALU.add),
             r=[k + 't1', k + 'ang'], w=[k + 't2'])
        if shift != 0.0:
            S.op('dve', lambda e: e.tensor_scalar(out=T['t2'][:], in0=T['t2'][:], scalar1=shift, scalar2=None, op0=ALU.add), r=[k + 't2'], w=[k + 't2'])
        S.op('dve', lambda e: e.tensor_scalar(out=T['t0'][:], in0=T['t2'][:], scalar1=-math.pi, scalar2=TWO_PI, op0=ALU.is_lt, op1=ALU.mult), r=[k + 't2'], w=[k + 't0'])
        S.op('dve', lambda e: e.tensor_tensor(out=T['t2'][:], in0=T['t2'][:], in1=T['t0'][:], op=ALU.add), r=[k + 't2', k + 't0'], w=[k + 't2'])
        S.op('dve', lambda e: e.tensor_scalar(out=T['t0'][:], in0=T['t2'][:], scalar1=math.pi, scalar2=-TWO_PI, op0=ALU.is_gt, op1=ALU.mult), r=[k + 't2'], w=[k + 't0'])
        S.op('dve', lambda e: e.tensor_tensor(out=T['t2'][:], in0=T['t2'][:], in1=T['t0'][:], op=ALU.add), r=[k + 't2', k + 't0'], w=[k + 't2'])
        S.op('dve', lambda e: e.tensor_scalar(out=T['t2'][:], in0=T['t2'][:], scalar1=-math.pi, scalar2=math.pi, op0=ALU.max, op1=ALU.min), r=[k + 't2'], w=[k + 't2'])
        S.op('act', lambda e: e.activation(out=T[dst][:], in_=T['t2'][:], func=AF.Sin), r=[k + 't2'], w=[k + dst])
        S.op('dve', lambda e: e.tensor_tensor(out=T[dst][:], in0=T[dst][:], in1=T['mag'][:], op=ALU.mult), r=[k + dst, k + 'mag'], w=[k + dst])
    return T


def build_s5(nc, S, sb, banks, BK, uT, bigA, bigB, p_l1, p_l2, c_l2, d_l1, ypreT, identf, E2, ngroups):
    P1 = sb("P1", [128, 5, 128]); P2 = sb("P2", [128, 3, 16]); C2 = sb("C2", [128, 16, 16]); D1 = sb("D1", [128, 1])
    S.dma('sp', lambda e: e.dma_start(out=P1[:], in_=p_l1[:, :, :]), w=['P1'])
    S.dma('sp', lambda e: e.dma_start(out=P2[:], in_=p_l2[:, :, :]), w=['P2'])
    S.dma('sp', lambda e: e.dma_start(out=C2[:], in_=c_l2[:, :, :]), w=['C2'])
    S.dma('sp', lambda e: e.dma_start(out=D1[:], in_=d_l1[:, :]), w=['D1'])
    T1 = complex_base(S, sb, 'c1', P1[:, 0, :], P1[:, 1, :], P1[:, 2, :], 128, ['P1'])
    den = sb("den", [128, 128]); nr = sb("nr", [128, 128]); cr = sb("cr", [128, 128]); ci = sb("ci", [128, 128]); tA = sb("tA", [128, 128]); tB = sb("tB", [128, 128])
    lrc, ar, ai = T1['lrc'], T1['ar'], T1['ai']
    li1 = P1[:, 1, :]
    tt = lambda o, a, b, op, r, w: S.op('dve', lambda e: e.tensor_tensor(out=o, in0=a, in1=b, op=op), r=r, w=w)
    tt(den[:], lrc[:], lrc[:], ALU.mult, ['c1lrc'], ['den'])
    tt(tA[:], li1, li1, ALU.mult, ['P1'], ['tA'])
    tt(den[:], den[:], tA[:], ALU.add, ['den', 'tA'], ['den'])
    S.op('dve', lambda e: e.reciprocal(out=den[:], in_=den[:]), r=['den'], w=['den'])
    S.op('dve', lambda e: e.tensor_scalar(out=nr[:], in0=ar[:], scalar1=-1.0, scalar2=None, op0=ALU.add), r=['c1ar'], w=['nr'])
    tt(tA[:], nr[:], lrc[:], ALU.mult, ['nr', 'c1lrc'], ['tA'])
    tt(tB[:], ai[:], li1, ALU.mult, ['c1ai', 'P1'], ['tB'])
    tt(cr[:], tA[:], tB[:], ALU.add, ['tA', 'tB'], ['cr'])
    tt(cr[:], cr[:], den[:], ALU.mult, ['cr', 'den'], ['cr'])
    tt(tA[:], ai[:], lrc[:], ALU.mult, ['c1ai', 'c1lrc'], ['tA'])
    tt(tB[:], nr[:], li1, ALU.mult, ['nr', 'P1'], ['tB'])
    tt(ci[:], tA[:], tB[:], ALU.subtract, ['tA', 'tB'], ['ci'])
    tt(ci[:], ci[:], den[:], ALU.mult, ['ci', 'den'], ['ci'])
    Ball = sb("Ball", [128, 2, 2, 64])
    br = P1[:, 3, :]; bi = P1[:, 4, :]
    tt(tA[:], cr[:], br, ALU.mult, ['cr', 'P1'], ['tA'])
    tt(tB[:], ci[:], bi, ALU.mult, ['ci', 'P1'], ['tB'])
    for d in range(2):
        tt(Ball[:, d, 0, :], tA[:, d * 64:(d + 1) * 64], tB[:, d * 64:(d + 1) * 64], ALU.subtract, ['tA', 'tB'], ['Ball'])
    tt(tA[:], cr[:], bi, ALU.mult, ['cr', 'P1'], ['tA'])
    tt(tB[:], ci[:], br, ALU.mult, ['ci', 'P1'], ['tB'])
    for d in range(2):
        tt(Ball[:, d, 1, :], tA[:, d * 64:(d + 1) * 64], tB[:, d * 64:(d + 1) * 64], ALU.add, ['tA', 'tB'], ['Ball'])
    gm = sb("gm", [128, 8])
    S.op('dve', lambda e: e.tensor_reduce(out=gm[:], in_=identf[:].rearrange("p (g c) -> p g c", c=16), axis=AX.X, op=ALU.add), r=['identf'], w=['gm'])
    T2 = complex_base(S, sb, 'c2', P2[:, 0, :], P2[:, 1, :], P2[:, 2, :], 16, ['P2'])
    NLEV = 5
    pw_r = [T2['ar']] + [sb("pwr%d" % l, [128, 16]) for l in range(1, NLEV)]
    pw_i = [T2['ai']] + [sb("pwi%d" % l, [128, 16]) for l in range(1, NLEV)]
    kr = lambda l: 'c2ar' if l == 0 else 'pwr%d' % l
    ki = lambda l: 'c2ai' if l == 0 else 'pwi%d' % l
    sqr = sb("sqr", [128, 16]); sqi = sb("sqi", [128, 16]); sqr2 = sb("sqr2", [128, 16]); sqi2 = sb("sqi2", [128, 16])
    for l in range(1, NLEV):
        src_r, src_i, skr, ski = pw_r[l - 1], pw_i[l - 1], kr(l - 1), ki(l - 1)
        for s_ in range(3):
            if s_ == 2:
                dr, di_, dkr, dki = pw_r[l], pw_i[l], kr(l), ki(l)
            elif s_ == 0:
                dr, di_, dkr, dki = sqr, sqi, 'sqr', 'sqi'
            else:
                dr, di_, dkr, dki = sqr2, sqi2, 'sqr2', 'sqi2'
            tt(tA[:, 0:16], src_r[:], src_r[:], ALU.mult, [skr], ['tA'])
            tt(tB[:, 0:16], src_i[:], src_i[:], ALU.mult, [ski], ['tB'])
            S.op('dve', lambda e: e.scalar_tensor_tensor(out=di_[:], in0=src_r[:], scalar=2.0, in1=src_i[:], op0=ALU.mult, op1=ALU.mult), r=[skr, ski], w=[dki])
            tt(dr[:], tA[:, 0:16], tB[:, 0:16], ALU.subtract, ['tA', 'tB'], [dkr])
            src_r, src_i, skr, ski = dr, di_, dkr, dki
    sA = [sb("sA%d" % l, [128, 16]) for l in range(NLEV)]
    sB = [sb("sB%d" % l, [128, 16]) for l in range(NLEV)]
    for l in range(NLEV):
        S.op('dve', lambda e: e.tensor_copy(out=sA[l][0:64, :], in_=pw_r[l][0:64, :]), r=[kr(l)], w=['sA%d' % l])
        S.op('dve', lambda e: e.tensor_scalar(out=sA[l][64:128, :], in0=pw_i[l][64:128, :], scalar1=-1.0, scalar2=None, op0=ALU.mult), r=[ki(l)], w=['sA%d' % l])
        S.op('dve', lambda e: e.tensor_copy(out=sB[l][0:64, :], in_=pw_i[l][0:64, :]), r=[ki(l)], w=['sB%d' % l])
        S.op('dve', lambda e: e.tensor_copy(out=sB[l][64:128, :], in_=pw_r[l][64:128, :]), r=[kr(l)], w=['sB%d' % l])
    Cb = sb("Cb", [128, 16, 16], BF16)
    S.op('dve', lambda e: e.tensor_copy(out=Cb[0:64], in_=C2[0:64]), r=['C2'], w=['Cb'])
    S.op('dve', lambda e: e.tensor_scalar(out=Cb[64:128], in0=C2[64:128], scalar1=-1.0, scalar2=None, op0=ALU.mult), r=['C2'], w=['Cb'])
    Dpad = sb("Dpad", [128, 128], BF16)
    S.op('dve', lambda e: e.tensor_scalar(out=Dpad[:], in0=identf[:], scalar1=D1[:, 0:1], scalar2=None, op0=ALU.mult), r=['identf', 'D1'], w=['Dpad'])

    X = {0: (bigA, 'bigA'), 1: (bigB, 'bigB')}
    Bpad = [sb("Bpad%d" % d, [128, 128], BF16) for d in range(2)]
    A0 = [sb("A0_%d" % d, [128, 128], BF16) for d in range(2)]
    Al = [[sb("A%d_%d" % (l, d), [128, 128]) for d in range(2)] for l in range(1, NLEV)]
    Hb = [sb("Hb%d" % d, [128, 2048], BF16) for d in range(2)]
    S1 = [sb("S1_%d" % d, [128, 2048]) for d in range(2)]
    S2 = [sb("S2_%d" % d, [128, 256]) for d in range(2)]
    S3 = [sb("S3_%d" % d, [128, 32]) for d in range(2)]
    S4 = [sb("S4_%d" % d, [128, 4]) for d in range(2)]
    S5t = [sb("S5_%d" % d, [128, 1]) for d in range(2)]
    Hf = [sb("Hf%d" % d, [128, 256]) for d in range(2)]
    P4 = [sb("P4_%d" % d, [128, 4]) for d in range(2)]
    P3 = [sb("P3_%d" % d, [128, 32]) for d in range(2)]
    P2_ = [sb("P2_%d" % d, [128, 256]) for d in range(2)]
    P1_ = [sb("P1_%d" % d, [128, 2048]) for d in range(2)]
    P1b = [sb("P1b_%d" % d, [128, 2048], BF16) for d in range(2)]
    ystage = [sb("ystage%d" % i, [16, 512]) for i in range(2)]
    Slev = [None, S1, S2, S3, S4, S5t]
    Plev = [None, P1_, P2_, P3, P4]
    Nlev = [L, 2048, 256, 32, 4, 1]
    import itertools
    for g in range(ngroups):
        def chain(d):
            col = d * 8 + g
            Xt, Xn = X[d]
            sfx = '_%d' % d
            S.op('pool', lambda e: e.tensor_scalar(out=Bpad[d][:], in0=Ball[:, d].rearrange("p r q -> p (r q)"), scalar1=gm[:, g:g + 1], scalar2=None, op0=ALU.mult),
                 r=['Ball', 'gm'], w=['Bpad' + sfx])
            for l in range(NLEV):
                At = A0[d] if l == 0 else Al[l - 1][d]
                ak = 'A%d' % l + sfx
                S.op('pool', lambda e: e.tensor_scalar(out=At[:, 0:64], in0=E2[:], scalar1=sA[l][:, col:col + 1], scalar2=None, op0=ALU.mult), r=['E2', 'sA%d' % l], w=[ak])
                S.op('pool', lambda e: e.tensor_scalar(out=At[:, 64:128], in0=E2[:], scalar1=sB[l][:, col:col + 1], scalar2=None, op0=ALU.mult), r=['E2', 'sB%d' % l], w=[ak])
            for blk in range(NBLK):
                t0 = blk * 512; bk = (blk % 2) if d == 0 else 4
                S.op('pe', lambda e: e.matmul(banks[bk][:], lhsT=Bpad[d][:], rhs=uT[:, t0:t0 + 512], start=True, stop=True),
                     r=['Bpad' + sfx] + KS('uT', t0, t0 + 512), w=[BK(bk)])
                S.op('act', lambda e: e.activation(out=Xt[:, t0:t0 + 512], in_=banks[bk][:], func=AF.Copy), r=[BK(bk)], w=KS(Xn, t0, t0 + 512))
                yield
            order = list(range(8)) if d == 0 else list(range(7, -1, -1))
            for k in range(1, 8):
                for q4 in range(4):
                    n0 = q4 * 512; bk = (q4 % 2 + 2) if d == 0 else (q4 % 2 + 5)
                    xkeys = KS(Xn, n0 * 8, (n0 + 512) * 8)
                    jp, j = order[k - 1], order[k]
                    rhs = Xt[:, n0 * 8 + jp:(n0 + 512) * 8:8] if k == 1 else Hb[d][:, n0:n0 + 512]
                    rk = xkeys if k == 1 else [('Hb' + sfx, q4)]
                    S.op('pe', lambda e: e.matmul(banks[bk][:], lhsT=A0[d][:], rhs=rhs, start=True, stop=True), r=['A0' + sfx] + rk, w=[BK(bk)])
                    if k < 7:
                        S.op('dve', lambda e: e.tensor_tensor(out=Hb[d][:, n0:n0 + 512], in0=banks[bk][:], in1=Xt[:, n0 * 8 + j:(n0 + 512) * 8:8], op=ALU.add),
                             r=[BK(bk)] + xkeys, w=[('Hb' + sfx, q4)])
                    else:
                        S.op('dve', lambda e: e.tensor_tensor(out=S1[d][:, n0:n0 + 512], in0=banks[bk][:], in1=Xt[:, n0 * 8 + j:(n0 + 512) * 8:8], op=ALU.add),
                             r=[BK(bk)] + xkeys, w=[('S1' + sfx, q4)])
                    yield
            for l in range(1, 5):
                R = 8 if l < 4 else 4
                Nn = Nlev[l + 1]
                src = Slev[l][d]; dst = Slev[l + 1][d]
                srck = [('S%d' % l + sfx, i) for i in range(4)] if l == 1 else ['S%d' % l + sfx]
                dstk = ['S%d' % (l + 1) + sfx]
                ordr = list(range(R)) if d == 0 else list(range(R - 1, -1, -1))
                bk = 2 if d == 0 else 5
                for k in range(1, R):
                    jp, j = ordr[k - 1], ordr[k]
                    rhs = src[:, jp::R] if k == 1 else Hf[d][:, 0:Nn]
                    rk = srck if k == 1 else ['Hf' + sfx]
                    S.op('pe', lambda e: e.matmul(banks[bk][:, 0:Nn], lhsT=Al[l - 1][d][:], rhs=rhs, start=True, stop=True), r=['A%d' % l + sfx] + rk, w=[BK(bk)])
                    o = Hf[d][:, 0:Nn] if k < R - 1 else dst[:, 0:Nn]
                    ok = ['Hf' + sfx] if k < R - 1 else dstk
                    S.op('dve', lambda e: e.tensor_tensor(out=o, in0=banks[bk][:, 0:Nn], in1=src[:, j::R], op=ALU.add), r=[BK(bk)] + srck, w=ok)
                    yield
            for l in range(4, 0, -1):
                R = 8 if l < 4 else 4
                Nn = Nlev[l + 1]
                Sl = Slev[l][d]; Pl = Plev[l][d]
                slk = [('S%d' % l + sfx, i) for i in range(4)] if l == 1 else ['S%d' % l + sfx]
                plk = 'P%d' % l + sfx
                ordr = list(range(R)) if d == 0 else list(range(R - 1, -1, -1))
                bk = 3 if d == 0 else 6
                if l == 4:
                    S.op('dve', lambda e: e.memset(Pl[:, ordr[0]:ordr[0] + 1], 0.0), w=[plk])
                else:
                    Pup = Plev[l + 1][d]
                    S.op('dve', lambda e: e.tensor_copy(out=Pl[:, ordr[0]::R], in_=Pup[:, 0:Nn]), r=['P%d' % (l + 1) + sfx], w=[plk])
                for k in range(1, R):
                    jp, j = ordr[k - 1], ordr[k]
                    S.op('pe', lambda e: e.matmul(banks[bk][:, 0:Nn], lhsT=Al[l - 1][d][:], rhs=Pl[:, jp::R], start=True, stop=True), r=['A%d' % l + sfx, plk], w=[BK(bk)])
                    S.op('dve', lambda e: e.tensor_tensor(out=Pl[:, j::R], in0=banks[bk][:, 0:Nn], in1=Sl[:, jp::R], op=ALU.add), r=[BK(bk)] + slk, w=[plk])
                    yield
            S.op('act', lambda e: e.activation(out=P1b[d][:], in_=P1_[d][:], func=AF.Copy), r=['P1' + sfx], w=['P1b' + sfx])
            for k in range(8):
                for q4 in range(4):
                    n0 = q4 * 512; bk = (q4 % 2 + 2) if d == 0 else (q4 % 2 + 5)
                    xkeys = KS(Xn, n0 * 8, (n0 + 512) * 8)
                    j = order[k]
                    if k == 0:
                        rhs = P1b[d][:, n0:n0 + 512]; rk = ['P1b' + sfx]
                    else:
                        jp = order[k - 1]
                        rhs = Xt[:, n0 * 8 + jp:(n0 + 512) * 8:8]; rk = xkeys
                    S.op('pe', lambda e: e.matmul(banks[bk][:], lhsT=A0[d][:], rhs=rhs, start=True, stop=True), r=['A0' + sfx] + rk, w=[BK(bk)])
                    xs = Xt[:, n0 * 8 + j:(n0 + 512) * 8:8]
                    S.op('dve', lambda e: e.tensor_tensor(out=xs, in0=banks[bk][:], in1=xs, op=ALU.add), r=[BK(bk)] + xkeys, w=xkeys)
                    yield
        for _ in itertools.zip_longest(chain(0), chain(1)):
            pass
        for blk in range(NBLK):
            t0 = blk * 512; bk = blk % 2; ys = ystage[blk % 2]; yk = 'ystage%d' % (blk % 2)
            S.op('pe', lambda e: e.matmul(banks[bk][0:16, :], lhsT=Cb[:, 0 * 8 + g, :], rhs=bigA[:, t0:t0 + 512], start=True, stop=False),
                 r=['Cb'] + KS('bigA', t0, t0 + 512), w=[BK(bk)])
            S.op('pe', lambda e: e.matmul(banks[bk][0:16, :], lhsT=Cb[:, 1 * 8 + g, :], rhs=bigB[:, t0:t0 + 512], start=False, stop=False),
                 r=['Cb'] + KS('bigB', t0, t0 + 512), w=[BK(bk)], pe_acc=True)
            S.op('pe', lambda e: e.matmul(banks[bk][0:16, :], lhsT=Dpad[:, 16 * g:16 * g + 16], rhs=uT[:, t0:t0 + 512], start=False, stop=True),
                 r=['Dpad'] + KS('uT', t0, t0 + 512), w=[BK(bk)], pe_acc=True)
            S.op('act', lambda e: e.activation(out=ys[:], in_=banks[bk][0:16, :], func=AF.Copy), r=[BK(bk)], w=[yk])
            S.dma('sp', lambda e: e.dma_start(out=ypreT[16 * g:16 * g + 16, t0:t0 + 512], in_=ys[:]), r=[yk])


def prep_l1_inputs(inp, b, q):
    x = inp["x"]; w_in = inp["ab_w_in"][0]
    xT = np.ascontiguousarray(x[b].T)
    ucols = w_in[:, q * 128:(q + 1) * 128]
    qc = w_in[:, 512 + q * 64:512 + (q + 1) * 64]
    kc = w_in[:, 768 + q * 64:768 + (q + 1) * 64]
    vc = w_in[:, 1024 + q * 128:1024 + (q + 1) * 128]
    goc = w_in[:, 1536 + q * 128:1536 + (q + 1) * 128]
    lrc = w_in[:, 2048:2080]
    w_fm = np.ascontiguousarray(np.concatenate([ucols, qc, qc, kc, kc, lrc], axis=1))
    w_tm = np.ascontiguousarray(np.concatenate([vc, goc], axis=1))
    gwf = inp["gla_gate_w"][0]
    gw = np.zeros((16, 256), np.float32)
    gw[:, 0:64] = gwf[0][:, q * 64:(q + 1) * 64]
    gw[:, 128 + 64:256] = gwf[1][:, q * 64:(q + 1) * 64]
    gbf = inp["gla_gate_b"][0]
    gb = np.concatenate([gbf[0][q * 64:(q + 1) * 64], gbf[1][q * 64:(q + 1) * 64]]).reshape(128, 1).astype(np.float32)
    ng = np.ascontiguousarray(np.broadcast_to(inp["gla_norm_g"][0][q][None, :], (128, 128))).astype(np.float32)
    gs = slice(q * 8, (q + 1) * 8)
    lam_re = inp["s5_lam_re"][0][:, gs]; lam_im = inp["s5_lam_im"][0][:, gs]; log_dt = inp["s5_log_dt"][0][:, gs]
    b_re = inp["s5_b_re"][0][:, gs]; b_im = inp["s5_b_im"][0][:, gs]
    c_re = inp["s5_c_re"][0][:, gs]; c_im = inp["s5_c_im"][0][:, gs]
    def l1_from_gp(a):
        t = np.transpose(a, (1, 0, 2))[:, None, :, :]
        t = np.broadcast_to(t, (8, 16, 2, 64))
        return t.reshape(128, 128)
    ldt_gp = np.broadcast_to(log_dt[:, :, None], (2, 8, 64))
    def l1_from_b(a):
        return np.transpose(a, (1, 3, 0, 2)).reshape(128, 128)
    p_l1 = np.stack([l1_from_gp(lam_re), l1_from_gp(lam_im), l1_from_gp(ldt_gp), l1_from_b(b_re), l1_from_b(b_im)], axis=1)
    def l2_from_gp(a):
        t = np.transpose(a, (2, 0, 1)).reshape(64, 16)
        return np.concatenate([t, t], axis=0)
    p_l2 = np.stack([l2_from_gp(lam_re), l2_from_gp(lam_im), l2_from_gp(ldt_gp)], axis=1)
    c2 = np.concatenate([np.transpose(c_re, (3, 0, 1, 2)).reshape(64, 16, 16), np.transpose(c_im, (3, 0, 1, 2)).reshape(64, 16, 16)], axis=0)
    d_l1 = inp["s5_d"][0][q * 128:(q + 1) * 128].reshape(128, 1)
    f = lambda a: np.ascontiguousarray(a, dtype=np.float32)
    return {"xT": f(xT), "w_fm": f(w_fm), "w_tm": f(w_tm), "gw": f(gw), "gb": f(gb), "ng": f(ng),
            "p_l1": f(p_l1), "p_l2": f(p_l2), "c_l2": f(c2), "d_l1": f(d_l1)}


NT = 4096
NTILE = NT // 128
ALPHA = 4.0 ** 0.25
HALO = 128
NTH = NT + 2 * HALO
NEG = -30000.0


def build_tail(mode, n_exp=32, ngrp_tok=2, p1stage=9, ntl=NTILE, do_fin=True):
    nc = bass.Bass("TRN2", target_bir_lowering=False)
    S = Sched(nc)
    din = lambda n, s, dt=F32: nc.dram_tensor(n, s, dt, kind="ExternalInput").ap()
    dout = lambda n, s, dt=F32: nc.dram_tensor(n, s, dt, kind="ExternalOutput").ap()
    if mode == 'ab':
        ypre = din("ypre", [NT, 512]); ybi = din("ybi", [NT, 512]); xin = din("xin", [NT, 1024])
        glu_w = din("glu_w", [512, 512]); glu_b = din("glu_b", [128, 512])
    else:
        hin = din("hin", [NTH, 1024])
        w_qkv = din("w_qkv", [1024, 1536])
        bias_full = din("bias_full", [128, 16, 384])
        kvalid = din("kvalid", [128, 2, 384])
        sink_bc = din("sink_bc", [128, 16])
    w_out = din("w_out", [1024, 1024])
    ln1 = din("ln1", [128, 2, 1024]); ln2 = din("ln2", [128, 2, 1024])
    w_r = din("w_r", [1024, 36]); b_r = din("b_r", [128, 36])
    w1 = din("w1", [32, 1024, 512]); w3 = din("w3", [32, 1024, 512]); w2 = din("w2", [32, 512, 1024])
    ebase_in = din("ebase_in", [128, 32])
    out = dout("out", [NT, 1024])
    CAP = 384
    Xs = nc.dram_tensor("Xs", [32 * CAP, 1024], BF16).ap()
    Ys = nc.dram_tensor("Ys", [32 * CAP, 1024], F32).ap()
    h1_sc = dout("h1_sc", [NT, 1024])

    sbp = lambda n, s, dt=F32: nc.alloc_sbuf_tensor(n, s, dt)
    banks = [nc.alloc_psum_tensor("bank%d" % i, [128, 512], F32) for i in range(6)]
    ptrs = [nc.alloc_psum_tensor("ptr_t%d" % i, [128, 1024], BF16) for i in range(2)]
    BK = lambda i: ('bank', i)

    identf = sbp("identf", [128, 128]); ident = sbp("ident", [128, 128], BF16)
    DEST = [[sbp("D_%d_%d" % (t_, k_), [128, 1], I32) for k_ in range(2)] for t_ in range(NTILE)]
    GATE = sbp("GATE", [128, NTILE, 2])
    ln2t = sbp("ln2t", [128, 2, 1024])
    c_eps = sbp("c_eps", [128, 1])
    dummy = sbp("fence_dummy", [128, 8])
    fence = lambda e: e.memset(dummy[:], 0.0)
    S.op('pool', lambda e: e.memset(identf[:], 0.0), w=['identf'])
    S.op('pool', lambda e: e.affine_select(out=identf[:], in_=identf[:], pattern=[[-1, 128]], compare_op=ALU.not_equal,
                                           fill=1.0, base=0, channel_multiplier=1), r=['identf'], w=['identf'])
    S.op('dve', lambda e: e.tensor_copy(out=ident[:], in_=identf[:]), r=['identf'], w=['ident'])
    S.op('pool', lambda e: e.memset(c_eps[:], 1e-5), w=['c_eps'])
    S.dma('sp', lambda e: e.dma_start(out=ln2t[:], in_=ln2[:, :, :]), w=['ln2t'])

    gs = ExitStack()
    sb = lambda n, s, dt=F32: gs.enter_context(nc.sbuf_tensor(n, s, dt))
    ln1t = sb("ln1t", [128, 2, 1024]); wo = sb("wo", [128, 8, 1024], BF16); wr = sb("wr", [128, 8, 36], BF16); brt = sb("brt", [128, 36])
    S.dma('sp', lambda e: e.dma_start(out=ln1t[:], in_=ln1[:, :, :]), w=['ln1t'])
    S.dma('pool', lambda e: e.dma_start(out=wo[:], in_=w_out.rearrange("(kt p) c -> p kt c", p=128)), w=['wo'])
    S.dma('pool', lambda e: e.dma_start(out=wr[:], in_=w_r.rearrange("(kt p) c -> p kt c", p=128)), w=['wr'])
    S.dma('sp', lambda e: e.dma_start(out=brt[:], in_=b_r[:, :]), w=['brt'])

    TriS = sb("TriS", [128, 128]); OnesM = sb("OnesM", [128, 128]); carry = sb("carry", [128, 32]); ebase = sb("ebase", [128, 32])
    dsf = sb("dsf", [128, 2])
    S.op('pool', lambda e: e.memset(OnesM[:], 1.0), w=['OnesM'])
    S.op('pool', lambda e: e.affine_select(out=TriS[:], in_=OnesM[:], pattern=[[1, 128]], compare_op=ALU.is_gt, fill=0.0, base=0, channel_multiplier=-1), r=['OnesM'], w=['TriS'])
    S.op('pool', lambda e: e.memset(carry[:], 0.0), w=['carry'])
    S.dma('sp', lambda e: e.dma_start(out=ebase[:], in_=ebase_in[:, :]), w=['ebase'])
    tp_cnt = [0]

    def transpose_to(dst_ap, src_ap, rkeys, wkeys):
        i = tp_cnt[0] % 2; tp_cnt[0] += 1
        pt_ = ptrs[i][:, 0:128]
        S.op('pe', lambda e: e.transpose(pt_, src_ap, ident[:]), r=list(rkeys) + ['ident'], w=['ptr%d' % i])
        eng = 'dve' if (tp_cnt[0] % 2) else 'act'
        if eng == 'dve':
            S.op('dve', lambda e: e.tensor_copy(out=dst_ap, in_=pt_), r=['ptr%d' % i], w=wkeys)
        else:
            S.op('act', lambda e: e.activation(out=dst_ap, in_=pt_, func=AF.Copy), r=['ptr%d' % i], w=wkeys)

    def transpose_group(dst3, srcs, rkeys, wkeys):
        n = len(srcs)
        i = tp_cnt[0] % 2; tp_cnt[0] += 1
        for j, src_ap in enumerate(srcs):
            S.op('pe', lambda e: e.transpose(ptrs[i][:, j * 128:(j + 1) * 128], src_ap, ident[:]), r=list(rkeys) + ['ident'], w=['ptr%d' % i], pe_acc=(j > 0))
        pv = ptrs[i][:, 0:n * 128].rearrange("p (n c) -> p n c", c=128)
        if tp_cnt[0] % 2:
            S.op('dve', lambda e: e.tensor_copy(out=dst3, in_=pv), r=['ptr%d' % i], w=wkeys)
        else:
            S.op('act', lambda e: e.activation(out=dst3, in_=pv, func=AF.Copy), r=['ptr%d' % i], w=wkeys)

    def layer_norm(r_t, rk, lnt, lnk, out_t, ok, tmp, tk, st, stk):
        S.op('pool', lambda e: e.memset(st[:], 0.0), w=[stk])
        S.op('act', lambda e: e.activation(out=tmp[:], in_=r_t[:], func=AF.Copy, accum_out=st[:, 0:1]), r=[rk, stk], w=[tk, stk])
        S.op('dve', lambda e: e.tensor_scalar(out=st[:, 1:2], in0=st[:, 0:1], scalar1=-1.0 / 1024.0, scalar2=None, op0=ALU.mult), r=[stk], w=[stk])
        S.op('dve', lambda e: e.tensor_scalar(out=r_t[:], in0=r_t[:], scalar1=st[:, 1:2], scalar2=None, op0=ALU.add), r=[rk, stk], w=[rk])
        S.op('act', lambda e: e.activation(out=tmp[:], in_=r_t[:], func=AF.Square, accum_out=st[:, 2:3]), r=[rk, stk], w=[tk, stk])
        S.op('act', lambda e: e.activation(out=st[:, 3:4], in_=st[:, 2:3], func=AF.Sqrt, bias=c_eps[:, 0:1], scale=1.0 / 1024.0), r=[stk, 'c_eps'], w=[stk])
        S.op('dve', lambda e: e.reciprocal(out=st[:, 3:4], in_=st[:, 3:4]), r=[stk], w=[stk])
        S.op('dve', lambda e: e.scalar_tensor_tensor(out=tmp[:], in0=r_t[:], scalar=st[:, 3:4], in1=lnt[:, 0, :], op0=ALU.mult, op1=ALU.mult), r=[rk, stk, lnk], w=[tk])
        S.op('pool', lambda e: e.tensor_tensor(out=out_t[:], in0=tmp[:], in1=lnt[:, 1, :], op=ALU.add), r=[tk, lnk], w=[ok])

    if mode == 'ab':
        gw = sb("gw", [128, 4, 512], BF16); gbt = sb("gbt", [128, 512])
        S.dma('pool', lambda e: e.dma_start(out=gw[:], in_=glu_w.rearrange("(kt p) c -> p kt c", p=128)), w=['gw'])
        S.dma('sp', lambda e: e.dma_start(out=gbt[:], in_=glu_b[:, :]), w=['gbt'])
        yp = [sb("yp%d" % i, [128, 512]) for i in range(2)]
        ybt = [sb("ybt%d" % i, [128, 512]) for i in range(2)]
        xt = [sb("xt%d" % i, [128, 1024]) for i in range(2)]
        ya0 = sb("ya0", [128, 512]); ya0b = sb("ya0b", [128, 512], BF16); ya0T = sb("ya0T", [128, 4, 128], BF16)
        sg = sb("sg", [128, 512]); cat = sb("cat", [128, 1024], BF16)
    else:
        wq = sb("wq", [128, 8, 1536], BF16)
        S.dma('pool', lambda e: e.dma_start(out=wq[:], in_=w_qkv.rearrange("(kt p) c -> p kt c", p=128)), w=['wq'])
        biasf = sb("biasf", [128, 16, 384]); kval = sb("kval", [128, 2, 384]); sinkt = sb("sinkt", [128, 16])
        S.dma('sp', lambda e: e.dma_start(out=biasf[:], in_=bias_full[:, :, :]), w=['biasf'])
        S.dma('sp', lambda e: e.dma_start(out=kval[:], in_=kvalid[:, :, :]), w=['kval'])
        S.dma('sp', lambda e: e.dma_start(out=sinkt[:], in_=sink_bc[:, :]), w=['sinkt'])
        kT = sb("kT", [128, 2, NTH], BF16)
        vtm = sb("vtm", [128, NTH // 128, 256], BF16)
        hb = [sb("hb%d" % i, [128, 1024], BF16) for i in range(2)]
        hTt = [sb("hTt%d" % i, [128, 8, 128], BF16) for i in range(2)]
        for t in range(NTH // 128):
            hbt = hb[t % 2]; hk = 'hb%d' % (t % 2); hT_ = hTt[t % 2]; htk = 'hTt%d' % (t % 2)
            S.dma('pool', lambda e: e.dma_start(out=hbt[:], in_=hin[t * 128:(t + 1) * 128, :]), w=[hk])
            transpose_group(hT_[:, :, :], [hbt[:, kt * 128:(kt + 1) * 128] for kt in range(8)], [hk], [htk])
            for pair in range(2):
                bk = pair
                for kt in range(8):
                    S.op('pe', lambda e: e.matmul(banks[bk][:, 0:128], lhsT=wq[:, kt, 1024 + pair * 128:1024 + (pair + 1) * 128], rhs=hT_[:, kt, :],
                                                  start=(kt == 0), stop=(kt == 7)), r=['wq', htk], w=[BK(bk)], pe_acc=(kt > 0))
                S.op('act', lambda e: e.activation(out=kT[:, pair, t * 128:(t + 1) * 128], in_=banks[bk][:, 0:128], func=AF.Copy), r=[BK(bk)], w=[('kT', t)])
            bk = 2 + t % 2
            for kt in range(8):
                S.op('pe', lambda e: e.matmul(banks[bk][:, 0:256], lhsT=hT_[:, kt, :], rhs=wq[:, kt, 1280:1536],
                                              start=(kt == 0), stop=(kt == 7)), r=['wq', htk], w=[BK(bk)], pe_acc=(kt > 0))
            S.op('dve', lambda e: e.tensor_copy(out=vtm[:, t, :], in_=banks[bk][:, 0:256]), r=[BK(bk)], w=[('vtm', t)])
        xt = [sb("xt%d" % i, [128, 1024]) for i in range(2)]
        qTh = [sb("qTh%d" % i, [128, 128], BF16) for i in range(6)]
        sc_t = [sb("sc%d" % i, [128, 384]) for i in range(6)]
        pb = [sb("pb%d" % i, [128, 384], BF16) for i in range(6)]
        pT = [sb("pT%d" % i, [128, 3, 128], BF16) for i in range(6)]
        mx = sb("mx", [128, 16, 4])
        cat = sb("cat", [128, 1024], BF16)
    catT = sb("catT", [128, 8, 128], BF16); h1Tt = sb("h1Tt", [128, 8, 128], BF16)
    rr = [sb("rr%d" % i, [128, 1024]) for i in range(2)]
    tmp = sb("tmp", [128, 1024]); h1f = [sb("h1f%d" % i, [128, 1024]) for i in range(2)]; h1b = sb("h1b", [128, 1024], BF16)
    st = [sb("st%d" % i, [128, 4]) for i in range(2)]
    lg = sb("lg", [128, 36]); rt = sb("rt", [128, 12, 32]); rs = sb("rs", [128, 16])

    for t in range(ntl):
        b2 = t % 2
        r0 = t * 128
        xk = 'xt%d' % b2
        if mode == 'ab':
            S.dma('sp', lambda e: e.dma_start(out=yp[b2][:], in_=ypre[r0:r0 + 128, :]), w=['yp%d' % b2])
            S.dma('sp', lambda e: e.dma_start(out=ybt[b2][:], in_=ybi[r0:r0 + 128, :]), w=['ybt%d' % b2])
            S.dma('sp', lambda e: e.dma_start(out=xt[b2][:], in_=xin[r0:r0 + 128, :]), w=[xk])
            S.op('act', lambda e: e.activation(out=ya0[:], in_=yp[b2][:], func=AF.Gelu_apprx_tanh), r=['yp%d' % b2], w=['ya0'])
            S.op('dve', lambda e: e.tensor_copy(out=ya0b[:], in_=ya0[:]), r=['ya0'], w=['ya0b'])
            transpose_group(ya0T[:, :, :], [ya0b[:, kt * 128:(kt + 1) * 128] for kt in range(4)], ['ya0b'], ['ya0T'])
            for kt in range(4):
                S.op('pe', lambda e: e.matmul(banks[0][:], lhsT=ya0T[:, kt, :], rhs=gw[:, kt, :], start=(kt == 0), stop=(kt == 3)), r=['ya0T', 'gw'], w=[BK(0)], pe_acc=(kt > 0))
            S.op('dve', lambda e: e.tensor_tensor(out=sg[:], in0=banks[0][:], in1=gbt[:], op=ALU.add), r=[BK(0), 'gbt'], w=['sg'])
            S.op('act', lambda e: e.activation(out=sg[:], in_=sg[:], func=AF.Sigmoid), r=['sg'], w=['sg'])
            S.op('dve', lambda e: e.tensor_tensor(out=cat[:, 0:512], in0=ya0[:], in1=sg[:], op=ALU.mult), r=['ya0', 'sg'], w=['cat'])
            S.op('pool', lambda e: e.tensor_copy(out=cat[:, 512:1024], in_=ybt[b2][:]), r=['ybt%d' % b2], w=['cat'])
        else:
            S.dma('sp', lambda e: e.dma_start(out=xt[b2][:], in_=hin[HALO + r0:HALO + r0 + 128, :]), w=[xk])
            k0 = r0
            tq = t + 1
            hbt = hb[t % 2]; hk = 'hb%d' % (t % 2); hT_ = hTt[t % 2]; htk = 'hTt%d' % (t % 2)
            S.dma('pool', lambda e: e.dma_start(out=hbt[:], in_=hin[tq * 128:(tq + 1) * 128, :]), w=[hk])
            transpose_group(hT_[:, :, :], [hbt[:, kt * 128:(kt + 1) * 128] for kt in range(8)], [hk], [htk])
            kkeys = [('kT', tq - 1), ('kT', tq), ('kT', tq + 1)]
            S.op('pool', lambda e: e.memset(mx[:], 0.0), w=[('mx', h_) for h_ in range(16)])
            import itertools

            def head_chain(h, hl):
                kv = h // 4; half = kv % 2; pair = kv // 2
                qt = qTh[hl]; qk = 'qTh%d' % hl
                bq = hl
                lo, hi = half * 64, (half + 1) * 64
                c_lo = h * 64 - half * 64
                mk = ('mx', h)
                for kt in range(8):
                    S.op('pe', lambda e: e.matmul(banks[bq][0:hi, 0:128], lhsT=wq[:, kt, c_lo:(h + 1) * 64], rhs=hT_[:, kt, :],
                                                  start=(kt == 0), stop=(kt == 7)), r=['wq', htk], w=[BK(bq)], pe_acc=(kt > 0))
                yield
                S.op('act', lambda e: e.activation(out=qt[lo:hi, :], in_=banks[bq][lo:hi, 0:128], func=AF.Copy, scale=0.125), r=[BK(bq)], w=[qk])
                yield
                S.op('pe', lambda e: e.matmul(banks[bq][:, 128:512], lhsT=qt[lo:hi, :], rhs=kT[lo:hi, pair, k0:k0 + 384], start=True, stop=True), r=[qk] + kkeys, w=[BK(bq)])
                yield
                sct = sc_t[hl]; sk = 'sc%d' % hl
                S.op('dve', lambda e: e.tensor_tensor(out=sct[:], in0=banks[bq][:, 128:512], in1=biasf[:, h, :], op=ALU.add), r=[BK(bq), 'biasf'], w=[sk])
                if t == 0 or t == NTILE - 1:
                    S.op('pool', lambda e: e.tensor_tensor(out=sct[:], in0=sct[:], in1=kval[:, 0 if t == 0 else 1, :], op=ALU.add), r=[sk, 'kval'], w=[sk])
                S.op('dve', lambda e: e.reduce_max(out=mx[:, h, 0:1], in_=sct[:], axis=AX.X), r=[sk], w=[mk])
                S.op('dve', lambda e: e.tensor_tensor(out=mx[:, h, 0:1], in0=mx[:, h, 0:1], in1=sinkt[:, h:h + 1], op=ALU.max), r=[mk, 'sinkt'], w=[mk])
                S.op('dve', lambda e: e.tensor_scalar(out=mx[:, h, 1:2], in0=mx[:, h, 0:1], scalar1=-1.0, scalar2=None, op0=ALU.mult), r=[mk], w=[mk])
                yield
                pbt = pb[hl]; pk = 'pb%d' % hl
                S.op('act', lambda e: e.activation(out=pbt[:], in_=sct[:], func=AF.Exp, bias=mx[:, h, 1:2], scale=1.0, accum_out=mx[:, h, 2:3]), r=[sk, mk], w=[pk, mk])
                S.op('act', lambda e: e.activation(out=mx[:, h, 3:4], in_=sinkt[:, h:h + 1], func=AF.Exp, bias=mx[:, h, 1:2], scale=1.0), r=['sinkt', mk], w=[mk])
                yield
                S.op('dve', lambda e: e.tensor_tensor(out=mx[:, h, 2:3], in0=mx[:, h, 2:3], in1=mx[:, h, 3:4], op=ALU.add), r=[mk], w=[mk])
                S.op('dve', lambda e: e.reciprocal(out=mx[:, h, 2:3], in_=mx[:, h, 2:3]), r=[mk], w=[mk])
                pTt = pT[hl]; ptk = 'pT%d' % hl
                transpose_group(pTt[:, :, :], [pbt[:, j * 128:(j + 1) * 128] for j in range(3)], [pk], [ptk])
                yield
                for j in range(3):
                    S.op('pe', lambda e: e.matmul(banks[bq][:, 0:64], lhsT=pTt[:, j, :], rhs=vtm[:, k0 // 128 + j, kv * 64:(kv + 1) * 64],
                                                  start=(j == 0), stop=(j == 2)), r=[ptk, ('vtm', k0 // 128 + j)], w=[BK(bq)], pe_acc=(j > 0))
                yield
                S.op('dve', lambda e: e.tensor_scalar(out=cat[:, h * 64:(h + 1) * 64], in0=banks[bq][:, 0:64], scalar1=mx[:, h, 2:3], scalar2=None, op0=ALU.mult),
                     r=[BK(bq), mk], w=[('cat', h)])
                yield

            for hs_ in ((0, 6), (6, 12), (12, 16)):
                for _ in itertools.zip_longest(*[head_chain(h_, h_ - hs_[0]) for h_ in range(hs_[0], hs_[1])]):
                    pass
        if p1stage < 2:
            continue
        catkeys = ['cat'] if mode == 'ab' else [('cat', h_) for h_ in range(16)]
        transpose_group(catT[:, :, :], [cat[:, kt * 128:(kt + 1) * 128] for kt in range(8)], catkeys, ['catT'])
        r_t = rr[b2]; rk = 'rr%d' % b2
        for half in range(2):
            bk = 4 + half
            for kt in range(8):
                S.op('pe', lambda e: e.matmul(banks[bk][:], lhsT=catT[:, kt, :], rhs=wo[:, kt, half * 512:(half + 1) * 512], start=(kt == 0), stop=(kt == 7)),
                     r=['catT', 'wo'], w=[BK(bk)], pe_acc=(kt > 0))
            S.op('dve', lambda e: e.scalar_tensor_tensor(out=r_t[:, half * 512:(half + 1) * 512], in0=xt[b2][:, half * 512:(half + 1) * 512], scalar=ALPHA,
                                                         in1=banks[bk][:], op0=ALU.mult, op1=ALU.add), r=[xk, BK(bk)], w=[rk])
        hf = h1f[b2]; hfk = 'h1f%d' % b2
        layer_norm(r_t, rk, ln1t, 'ln1t', hf, hfk, tmp, 'tmp', st[b2], 'st%d' % b2)
        S.dma('sp', lambda e: e.dma_start(out=h1_sc[r0:r0 + 128, :], in_=hf[:]), r=[hfk], w=[('h1_sc', t)])
        S.op('act', lambda e: e.activation(out=h1b[:], in_=hf[:], func=AF.Copy), r=[hfk], w=['h1b'])
        transpose_group(h1Tt[:, :, :], [h1b[:, kt * 128:(kt + 1) * 128] for kt in range(8)], ['h1b'], ['h1Tt'])
        if p1stage < 3:
            continue
        for kt in range(8):
            S.op('pe', lambda e: e.matmul(banks[4][:, 0:36], lhsT=h1Tt[:, kt, :], rhs=wr[:, kt, :], start=(kt == 0), stop=(kt == 7)),
                 r=['h1Tt', 'wr'], w=[BK(4)], pe_acc=(kt > 0))
        S.op('dve', lambda e: e.tensor_tensor(out=lg[:], in0=banks[4][:, 0:36], in1=brt[:], op=ALU.add), r=[BK(4), 'brt'], w=['lg'])
        dv = lambda fn, r, w: S.op('dve', fn, r=r, w=w)
        pv = lambda fn, r, w: S.op('pool', fn, r=r, w=w)
        dv(lambda e: e.reduce_max(out=rs[:, 0:1], in_=lg[:, 0:4], axis=AX.X), ['lg'], ['rs'])
        dv(lambda e: e.tensor_scalar(out=rs[:, 1:2], in0=rs[:, 0:1], scalar1=-1.0, scalar2=None, op0=ALU.mult), ['rs'], ['rs'])
        S.op('pool', lambda e: e.memset(rs[:, 2:3], 0.0), r=['rs'], w=['rs'])
        S.op('act', lambda e: e.activation(out=rt[:, 0, 0:4], in_=lg[:, 0:4], func=AF.Exp, bias=rs[:, 1:2], scale=1.0, accum_out=rs[:, 2:3]), r=['lg', 'rs'], w=['rt', 'rs'])
        dv(lambda e: e.reciprocal(out=rs[:, 3:4], in_=rs[:, 2:3]), ['rs'], ['rs'])
        dv(lambda e: e.tensor_scalar(out=rt[:, 1, 0:4], in0=lg[:, 0:4], scalar1=rs[:, 0:1], scalar2=None, op0=ALU.is_equal), ['lg', 'rs'], ['rt'])
        dv(lambda e: e.tensor_scalar(out=rt[:, 1, 0:4], in0=rt[:, 1, 0:4], scalar1=-1.0, scalar2=1e9, op0=ALU.add, op1=ALU.mult), ['rt'], ['rt'])
        for g_ in range(4):
            dv(lambda e: e.tensor_scalar(out=rt[:, 2, g_ * 8:(g_ + 1) * 8], in0=lg[:, 4 + g_ * 8:4 + (g_ + 1) * 8], scalar1=rt[:, 1, g_:g_ + 1], scalar2=None, op0=ALU.add), ['lg', 'rt'], ['rt'])
        dv(lambda e: e.reduce_max(out=rs[:, 4:5], in_=rt[:, 2, :], axis=AX.X), ['rt'], ['rs'])
        dv(lambda e: e.tensor_scalar(out=rt[:, 3, :], in0=rt[:, 2, :], scalar1=rs[:, 4:5], scalar2=None, op0=ALU.is_equal), ['rt', 'rs'], ['rt'])
        dv(lambda e: e.scalar_tensor_tensor(out=rt[:, 4, :], in0=rt[:, 3, :], scalar=-1e9, in1=rt[:, 2, :], op0=ALU.mult, op1=ALU.add), ['rt'], ['rt'])
        dv(lambda e: e.reduce_max(out=rs[:, 5:6], in_=rt[:, 4, :], axis=AX.X), ['rt'], ['rs'])
        dv(lambda e: e.tensor_scalar(out=rt[:, 5, :], in0=rt[:, 4, :], scalar1=rs[:, 5:6], scalar2=None, op0=ALU.is_equal), ['rt', 'rs'], ['rt'])
        dv(lambda e: e.tensor_tensor(out=rs[:, 6:7], in0=rs[:, 5:6], in1=rs[:, 4:5], op=ALU.subtract), ['rs'], ['rs'])
        S.op('act', lambda e: e.activation(out=rs[:, 6:7], in_=rs[:, 6:7], func=AF.Exp), r=['rs'], w=['rs'])
        dv(lambda e: e.tensor_scalar(out=rs[:, 6:7], in0=rs[:, 6:7], scalar1=1.0, scalar2=None, op0=ALU.add), ['rs'], ['rs'])
        dv(lambda e: e.reciprocal(out=rs[:, 7:8], in_=rs[:, 6:7]), ['rs'], ['rs'])
        dv(lambda e: e.tensor_tensor(out=rs[:, 7:8], in0=rs[:, 7:8], in1=rs[:, 3:4], op=ALU.mult), ['rs'], ['rs'])
        dv(lambda e: e.tensor_tensor(out=rs[:, 8:9], in0=rs[:, 3:4], in1=rs[:, 7:8], op=ALU.subtract), ['rs'], ['rs'])
        S.op('pool', lambda e: e.tensor_copy(out=GATE[:, t, :], in_=rs[:, 7:9]), r=['rs'], w=[('GATE', t)])
        dv(lambda e: e.tensor_tensor(out=rt[:, 7, :], in0=rt[:, 3, :], in1=rt[:, 5, :], op=ALU.add), ['rt'], ['rt'])
        S.op('pe', lambda e: e.matmul(banks[2][:, 0:32], lhsT=TriS[:], rhs=rt[:, 7, :], start=True, stop=True), r=['TriS', 'rt'], w=[BK(2)])
        S.op('pe', lambda e: e.matmul(banks[3][:, 0:32], lhsT=OnesM[:], rhs=rt[:, 7, :], start=True, stop=True), r=['OnesM', 'rt'], w=[BK(3)])
        dv(lambda e: e.tensor_tensor(out=rt[:, 8, :], in0=banks[2][:, 0:32], in1=carry[:], op=ALU.add), [BK(2), 'carry'], ['rt'])
        dv(lambda e: e.tensor_scalar(out=rt[:, 8, :], in0=rt[:, 8, :], scalar1=float(CAP - 1), scalar2=None, op0=ALU.min), ['rt'], ['rt'])
        dv(lambda e: e.tensor_tensor(out=rt[:, 8, :], in0=rt[:, 8, :], in1=ebase[:], op=ALU.add), ['rt', 'ebase'], ['rt'])
        dv(lambda e: e.tensor_tensor(out=carry[:], in0=carry[:], in1=banks[3][:, 0:32], op=ALU.add), [BK(3), 'carry'], ['carry'])
        dv(lambda e: e.tensor_tensor(out=rt[:, 9, :], in0=rt[:, 8, :], in1=rt[:, 3, :], op=ALU.mult), ['rt'], ['rt'])
        dv(lambda e: e.reduce_sum(out=dsf[:, 0:1], in_=rt[:, 9, :], axis=AX.X), ['rt'], ['dsf'])
        dv(lambda e: e.tensor_tensor(out=rt[:, 10, :], in0=rt[:, 8, :], in1=rt[:, 5, :], op=ALU.mult), ['rt'], ['rt'])
        dv(lambda e: e.reduce_sum(out=dsf[:, 1:2], in_=rt[:, 10, :], axis=AX.X), ['rt'], ['dsf'])
        for k_ in range(2):
            dv(lambda e: e.tensor_copy(out=DEST[t][k_][:], in_=dsf[:, k_:k_ + 1]), ['dsf'], [('DEST', t)])
        for k_ in range(2):
            tk_ = S.dma('pool', lambda e: e.indirect_dma_start(out=Xs[:, :], out_offset=bass.IndirectOffsetOnAxis(ap=DEST[t][k_][:, :], axis=0), in_=h1b[:, :], in_offset=None), r=['h1b', ('DEST', t)], w=['Xs'])

    S.barrier()
    gs.close()

    gs2 = ExitStack()
    sb2 = lambda n, s, dt=F32: gs2.enter_context(nc.sbuf_tensor(n, s, dt))
    NST = CAP // 128
    w1b = [sb2("w1b%d" % i, [128, 8, 512], BF16) for i in range(4)]
    w3b = [sb2("w3b%d" % i, [128, 8, 512], BF16) for i in range(4)]
    w2b = [sb2("w2b%d" % i, [128, 4, 1024], BF16) for i in range(4)]
    xe = [sb2("xe%d" % i, [128, NST, 1024], BF16) for i in range(2)]
    xeT = [sb2("xeT%d" % i, [128, 8, CAP], BF16) for i in range(2)]
    sil = [sb2("sil%d" % i, [128, CAP]) for i in range(2)]
    hdT = [sb2("hdT%d" % i, [128, 4, CAP], BF16) for i in range(2)]
    ye = [sb2("ye%d" % i, [128, 1024]) for i in range(2)]
    fin = [sb2("fin%d" % i, [128, 1024]) for i in range(2)]
    y1 = [sb2("y1_%d" % i, [128, 1024]) for i in range(2)]
    y2 = [sb2("y2_%d" % i, [128, 1024]) for i in range(2)]
    fo = [sb2("fo%d" % i, [128, 1024]) for i in range(2)]
    tmp2 = sb2("tmp2", [128, 1024]); st2 = [sb2("st2_%d" % i, [128, 4]) for i in range(2)]
    import itertools

    def expert_chain(ex):
        wb = ex % 4; par = ex % 2
        S.dma('pool', lambda e: e.dma_start(out=w1b[wb][:], in_=w1[ex].rearrange("(kt p) c -> p kt c", p=128)), w=['w1b%d' % wb])
        S.dma('pool', lambda e: e.dma_start(out=w3b[wb][:], in_=w3[ex].rearrange("(kt p) c -> p kt c", p=128)), w=['w3b%d' % wb])
        S.dma('pool', lambda e: e.dma_start(out=w2b[wb][:], in_=w2[ex].rearrange("(kt p) c -> p kt c", p=128)), w=['w2b%d' % wb])
        xet = xe[par]; xek = 'xe%d' % par; xT_ = xeT[par]; xTk = 'xeT%d' % par
        S.dma('sp', lambda e: e.dma_start(out=xet[:], in_=Xs[ex * CAP:(ex + 1) * CAP, :].rearrange("(s p) c -> p s c", p=128)), r=['Xs'], w=[xek])
        yield
        for st_ in range(NST):
            transpose_group(xT_[:, :, st_ * 128:(st_ + 1) * 128], [xet[:, st_, kt * 128:(kt + 1) * 128] for kt in range(8)], [xek], [xTk])
            yield
        hd = hdT[par]; hdk = 'hdT%d' % par
        b1 = 0 + par; b3 = 2 + par; by = 4 + par
        for ht in range(4):
            for kt in range(8):
                S.op('pe', lambda e: e.matmul(banks[b1][:, 0:CAP], lhsT=w1b[wb][:, kt, ht * 128:(ht + 1) * 128], rhs=xT_[:, kt, :], start=(kt == 0), stop=(kt == 7)),
                     r=['w1b%d' % wb, xTk], w=[BK(b1)], pe_acc=(kt > 0))
            yield
            for kt in range(8):
                S.op('pe', lambda e: e.matmul(banks[b3][:, 0:CAP], lhsT=w3b[wb][:, kt, ht * 128:(ht + 1) * 128], rhs=xT_[:, kt, :], start=(kt == 0), stop=(kt == 7)),
                     r=['w3b%d' % wb, xTk], w=[BK(b3)], pe_acc=(kt > 0))
            sl = sil[par]; slk = 'sil%d' % par
            S.op('act', lambda e: e.activation(out=sl[:], in_=banks[b1][:, 0:CAP], func=AF.Silu), r=[BK(b1)], w=[slk])
            yield
            S.op('dve', lambda e: e.tensor_tensor(out=hd[:, ht, :], in0=banks[b3][:, 0:CAP], in1=sl[:], op=ALU.mult), r=[BK(b3), slk], w=[(hdk, ht)])
            yield
        for st_ in range(NST):
            yet = ye[par]; yek = 'ye%d' % par
            for half in range(2):
                for ht in range(4):
                    S.op('pe', lambda e: e.matmul(banks[by][:], lhsT=hd[:, ht, st_ * 128:(st_ + 1) * 128], rhs=w2b[wb][:, ht, half * 512:(half + 1) * 512], start=(ht == 0), stop=(ht == 3)),
                         r=[(hdk, ht), 'w2b%d' % wb], w=[BK(by)], pe_acc=(ht > 0))
                yield
                if half == 0:
                    S.op('dve', lambda e: e.tensor_copy(out=yet[:, 0:512], in_=banks[by][:]), r=[BK(by)], w=[yek])
                else:
                    S.op('act', lambda e: e.activation(out=yet[:, 512:1024], in_=banks[by][:], func=AF.Copy), r=[BK(by)], w=[yek])
                yield
            r0 = ex * CAP + st_ * 128
            S.dma('sp', lambda e: e.dma_start(out=Ys[r0:r0 + 128, :], in_=yet[:]), r=[yek], w=['Ys'])
            yield

    for ex0 in range(0, n_exp, 2):
        for _ in itertools.zip_longest(expert_chain(ex0), expert_chain(ex0 + 1)):
            pass
    S.barrier()
    for t in range(NTILE):
        b2 = t % 2; r0 = t * 128
        S.dma('sp', lambda e: e.dma_start(out=fin[b2][:], in_=h1_sc[r0:r0 + 128, :]), r=[('h1_sc', t)], w=['fin%d' % b2])
        tk_ = S.dma('pool', lambda e: e.indirect_dma_start(out=y1[b2][:, :], out_offset=None, in_=Ys[:, :], in_offset=bass.IndirectOffsetOnAxis(ap=DEST[t][0][:, :], axis=0)), r=['Ys', ('DEST', t)], w=['y1_%d' % b2])
        tk_ = S.dma('pool', lambda e: e.indirect_dma_start(out=y2[b2][:, :], out_offset=None, in_=Ys[:, :], in_offset=bass.IndirectOffsetOnAxis(ap=DEST[t][1][:, :], axis=0)), r=['Ys', ('DEST', t)], w=['y2_%d' % b2])
        S.op('dve', lambda e: e.scalar_tensor_tensor(out=y1[b2][:], in0=y1[b2][:], scalar=GATE[:, t, 0:1], in1=y1[b2][:], op0=ALU.mult, op1=ALU.bypass),
             r=['y1_%d' % b2, ('GATE', t)], w=['y1_%d' % b2]) if False else None
        S.op('dve', lambda e: e.tensor_scalar(out=y1[b2][:], in0=y1[b2][:], scalar1=GATE[:, t, 0:1], scalar2=None, op0=ALU.mult), r=['y1_%d' % b2, ('GATE', t)], w=['y1_%d' % b2])
        S.op('dve', lambda e: e.scalar_tensor_tensor(out=y1[b2][:], in0=y2[b2][:], scalar=GATE[:, t, 1:2], in1=y1[b2][:], op0=ALU.mult, op1=ALU.add),
             r=['y1_%d' % b2, 'y2_%d' % b2, ('GATE', t)], w=['y1_%d' % b2])
        S.op('dve', lambda e: e.scalar_tensor_tensor(out=fin[b2][:], in0=fin[b2][:], scalar=ALPHA, in1=y1[b2][:], op0=ALU.mult, op1=ALU.add),
             r=['fin%d' % b2, 'y1_%d' % b2], w=['fin%d' % b2])
        layer_norm(fin[b2], 'fin%d' % b2, ln2t, 'ln2t', fo[b2], 'fo%d' % b2, tmp2, 'tmp2', st2[b2], 'st2_%d' % b2)
        S.dma('sp', lambda e: e.dma_start(out=out[r0:r0 + 128, :], in_=fo[b2][:]), r=['fo%d' % b2])
    S.wait_all('sp')
    print("instr counts", S.cnt, "waits", S.nwaits)
    return nc


def bc(a, n=128):
    return np.ascontiguousarray(np.broadcast_to(a[None], (n,) + a.shape)).astype(np.float32)


def common_inputs(inp, layer):
    f = lambda a: np.ascontiguousarray(a, dtype=np.float32)
    return {
        "ln1": f(np.stack([bc(inp["ln_mix_g"][layer]), bc(inp["ln_mix_b"][layer])], axis=1)),
        "ln2": f(np.stack([bc(inp["ln_ffn_g"][layer]), bc(inp["ln_ffn_b"][layer])], axis=1)),
        "w_r": f(np.concatenate([inp["moe_w_group"][layer], inp["moe_w_router"][layer]], axis=1)),
        "b_r": f(bc(np.concatenate([inp["moe_b_group"][layer], inp["moe_b_router"][layer]]))),
        "ebase_in": f(bc(np.arange(32, dtype=np.float32) * 384.0)),
        "w1": f(inp["moe_w1"][layer]), "w3": f(inp["moe_w3"][layer]), "w2": f(inp["moe_w2"][layer]),
    }


def t5_bucket_np(rel):
    nb = 16; max_exact = 8
    ret = (rel > 0).astype(np.int32) * nb
    n = np.abs(rel)
    nf = np.maximum(n, 1).astype(np.float32) / np.float32(max_exact)
    large = max_exact + (np.log(nf).astype(np.float32) / np.float32(math.log(128 / max_exact)) * np.float32(nb - max_exact)).astype(np.int32)
    large = np.minimum(large, nb - 1)
    return ret + np.where(n < max_exact, n, large)


_NC_CACHE = {}


def _get_nc(key, fn):
    if key not in _NC_CACHE:
        _NC_CACHE[key] = fn()
    return _NC_CACHE[key]


def kernel(**inputs):
    inp = {k: np.asarray(v) for k, v in inputs.items()}
    B, Lq = 2, 16384
    nc1 = build_l1()
    maps = [prep_l1_inputs(inp, c // 4, c % 4) for c in range(8)]
    res = run_bass_kernel_spmd(nc1, maps, core_ids=list(range(8)))
    ypre = np.zeros((B, Lq, 512), np.float32); yb = np.zeros((B, Lq, 512), np.float32)
    for c in range(8):
        b, q = c // 4, c % 4
        ypre[b, :, q * 128:(q + 1) * 128] = res.results[c]["ypreT"].T
        yb[b, :, q * 128:(q + 1) * 128] = res.results[c]["yb"]
    del res, maps
    nc2 = build_tail('ab')
    com = common_inputs(inp, 0)
    maps = []
    for c in range(8):
        b, s = c // 4, c % 4
        sl = slice(s * NT, (s + 1) * NT)
        m = dict(com)
        m.update({"ypre": np.ascontiguousarray(ypre[b, sl]), "ybi": np.ascontiguousarray(yb[b, sl]),
                  "xin": np.ascontiguousarray(inp["x"][b, sl], dtype=np.float32),
                  "glu_w": np.ascontiguousarray(inp["s5_glu_w"][0], dtype=np.float32), "glu_b": bc(inp["s5_glu_b"][0]),
                  "w_out": np.ascontiguousarray(inp["ab_w_out"][0], dtype=np.float32)})
        maps.append(m)
    res = run_bass_kernel_spmd(nc2, maps, core_ids=list(range(8)))
    h2 = np.zeros((B, Lq, 1024), np.float32)
    for c in range(8):
        b, s = c // 4, c % 4
        h2[b, s * NT:(s + 1) * NT] = res.results[c]["out"]
    del res, maps, ypre, yb
    nc3 = build_tail('attn')
    com = common_inputs(inp, 1)
    ii = np.arange(128)[:, None]; kk = np.arange(384)[None, :]
    rel = kk - 128 - ii
    bucket = t5_bucket_np(rel)
    rb = inp["rel_bias"].astype(np.float32)[bucket]
    band = (np.abs(rel) <= 128)[:, :, None]
    bias_full = np.where(band, rb, np.float32(NEG)).astype(np.float32).transpose(0, 2, 1)
    bias_full = np.ascontiguousarray(bias_full)
    sink_bc = bc(inp["c_sink"][0].astype(np.float32))
    h2p = np.zeros((B, Lq + 2 * HALO, 1024), np.float32)
    h2p[:, HALO:HALO + Lq] = h2
    maps = []
    for c in range(8):
        b, s = c // 4, c % 4
        m = dict(com)
        kvalid = np.zeros((128, 2, 384), np.float32)
        if s == 0:
            kvalid[:, 0, 0:128] = NEG
        if s == 3:
            kvalid[:, 1, 256:384] = NEG
        m.update({"hin": np.ascontiguousarray(h2p[b, s * NT:s * NT + NTH]), "w_qkv": np.ascontiguousarray(inp["c_w_in"][0], dtype=np.float32),
                  "bias_full": bias_full, "kvalid": kvalid, "sink_bc": sink_bc,
                  "w_out": np.ascontiguousarray(inp["c_w_out"][0], dtype=np.float32)})
        maps.append(m)
    res = run_bass_kernel_spmd(nc3, maps, core_ids=list(range(8)))
    out = np.zeros((B, Lq, 1024), np.float32)
    for c in range(8):
        b, s = c // 4, c % 4
        out[b, s * NT:(s + 1) * NT] = res.results[c]["out"]
    return out
```
